# Optimizing a Trainium2 kernel written in Bass

```python
import math
import jax
import jax.numpy as jnp
from jax import lax
import numpy as np

D_MODEL = 1024
BATCH = 16
SEQ = 4096
DEPTH = 1

ATT_HEADS = 8
HEAD_DIM = 64
ATT_WIDTH = ATT_HEADS * HEAD_DIM
MOBA_BLOCK = 256
MOBA_TOPK = 3
QUERY_CHUNK = 16
NUM_BUCKETS = 32
MAX_DISTANCE = 128
SSM_WIDTH = 256
SSM_GROUP = 16
SSM_GROUPS = SSM_WIDTH // SSM_GROUP
SSM_STATE = 64
DT_MIN = 0.001
DT_MAX = 0.1
IN_SPLITS = (ATT_WIDTH, ATT_WIDTH, ATT_WIDTH, SSM_WIDTH, D_MODEL, D_MODEL)
IN_WIDTH = sum(IN_SPLITS)
N_GROUPS = 4
EXPERTS_PER_GROUP = 8
N_EXPERTS = N_GROUPS * EXPERTS_PER_GROUP
EXPERT_TOPK = 2
D_EXPERT = D_MODEL // 2
MOE_BLOCK = 256
ADA_CHUNKS = 6
RMS_EPS = 1e-6
NEG_INF = -1e30

kernel_name = "hybrid_moba_s5_hiermoe_block"


def rms_norm(x, gain):
    xf = x.astype(jnp.float32)
    y = xf * lax.rsqrt(jnp.mean(xf * xf, axis=-1, keepdims=True) + RMS_EPS)
    return (y * gain.astype(jnp.float32)).astype(x.dtype)


def t5_bucket(dist):
    n = jnp.maximum(dist, 0)
    max_exact = NUM_BUCKETS // 2
    nf = jnp.maximum(n, 1).astype(jnp.float32)
    large = max_exact + (jnp.log(nf / max_exact) / math.log(MAX_DISTANCE / max_exact)
                         * (NUM_BUCKETS - max_exact)).astype(jnp.int32)
    large = jnp.minimum(large, NUM_BUCKETS - 1)
    return jnp.where(n < max_exact, n, large)


def moba_attention(q, k, v, rel_bias):
    bsz, s, nh, dh = q.shape
    nb = -(-s // MOBA_BLOCK)
    s_pad = nb * MOBA_BLOCK
    n_chunks = s_pad // QUERY_CHUNK
    k_sel = min(MOBA_TOPK, nb)
    pad = ((0, 0), (0, s_pad - s), (0, 0), (0, 0))
    q, k, v = [jnp.pad(t, pad).transpose(0, 2, 1, 3) for t in (q, k, v)]
    kb = k.reshape(bsz, nh, nb, MOBA_BLOCK, dh)
    vb = v.reshape(bsz, nh, nb, MOBA_BLOCK, dh)
    k_mean = jnp.mean(kb.astype(jnp.float32), axis=3)
    gate = jnp.einsum("bhsd,bhnd->bhsn", q.astype(jnp.float32), k_mean)
    q_blk = jnp.arange(s_pad) // MOBA_BLOCK
    past = jnp.arange(nb)[None, :] < q_blk[:, None]
    gate = jnp.where(past, gate, NEG_INF)
    _, sel = lax.top_k(gate, k_sel)
    sel_valid = sel < q_blk[:, None]

    def to_chunks(t):
        t = t.reshape(bsz, nh, n_chunks, QUERY_CHUNK, *t.shape[3:])
        return jnp.moveaxis(t, 2, 0)

    head_bias = rel_bias.T.astype(jnp.float32)
    b_ix = jnp.arange(bsz)[:, None, None, None]
    h_ix = jnp.arange(nh)[None, :, None, None]
    h_ix5 = jnp.arange(nh)[None, :, None, None, None]
    offs = jnp.arange(MOBA_BLOCK)
    scale = HEAD_DIM ** -0.5

    def chunk_attend(args):
        qc, selc, validc, ci = args
        q_pos = ci * QUERY_CHUNK + jnp.arange(QUERY_CHUNK)
        own = (ci * QUERY_CHUNK) // MOBA_BLOCK
        kg = kb[b_ix, h_ix, selc]
        vg = vb[b_ix, h_ix, selc]
        key_pos = selc[..., None] * MOBA_BLOCK + offs
        bucket = t5_bucket(q_pos[None, None, :, None, None] - key_pos)
        bias_sel = head_bias[h_ix5, bucket]
        s_sel = jnp.einsum("bhqd,bhqnkd->bhqnk", qc, kg).astype(jnp.float32) * scale + bias_sel
        s_sel = jnp.where(validc[..., None], s_sel, NEG_INF).reshape(bsz, nh, QUERY_CHUNK, k_sel * MOBA_BLOCK)
        k_own = lax.dynamic_index_in_dim(kb, own, axis=2, keepdims=False)
        v_own = lax.dynamic_index_in_dim(vb, own, axis=2, keepdims=False)
        dist = q_pos[:, None] - (own * MOBA_BLOCK + offs)[None, :]
        bias_own = head_bias[:, t5_bucket(dist)]
        s_own = jnp.einsum("bhqd,bhkd->bhqk", qc, k_own).astype(jnp.float32) * scale + bias_own
        s_own = jnp.where(dist >= 0, s_own, NEG_INF)
        p = jax.nn.softmax(jnp.concatenate([s_sel, s_own], axis=-1), axis=-1)
        p_sel = p[..., :k_sel * MOBA_BLOCK].reshape(bsz, nh, QUERY_CHUNK, k_sel, MOBA_BLOCK).astype(vg.dtype)
        p_own = p[..., k_sel * MOBA_BLOCK:].astype(v_own.dtype)
        return (jnp.einsum("bhqnk,bhqnkd->bhqd", p_sel, vg)
                + jnp.einsum("bhqk,bhkd->bhqd", p_own, v_own))

    out = lax.map(chunk_attend, (to_chunks(q), to_chunks(sel), to_chunks(sel_valid),
                                 jnp.arange(n_chunks)))
    out = jnp.moveaxis(out, 0, 2).reshape(bsz, nh, s_pad, dh)[:, :, :s]
    return out.transpose(0, 2, 1, 3).reshape(bsz, s, nh * dh)


def s5_ssm(u, lam_re, lam_im, log_dt, b_re, b_im, c_re, c_im, d_skip):
    bsz, s, _ = u.shape
    uf = u.astype(jnp.float32).reshape(bsz, s, SSM_GROUPS, SSM_GROUP)
    dt = jnp.exp(log_dt.astype(jnp.float32))[:, None]
    lr = lam_re.astype(jnp.float32)
    li = lam_im.astype(jnp.float32)
    mag = jnp.exp(lr * dt)
    ab_re = mag * jnp.cos(li * dt)
    ab_im = mag * jnp.sin(li * dt)
    den = lr * lr + li * li
    nr = ab_re - 1.0
    ni = ab_im
    f_re = (nr * lr + ni * li) / den
    f_im = (ni * lr - nr * li) / den
    br = b_re.astype(jnp.float32)
    bi = b_im.astype(jnp.float32)
    bb_re = f_re[..., None] * br - f_im[..., None] * bi
    bb_im = f_re[..., None] * bi + f_im[..., None] * br
    bu_re = jnp.einsum("gpc,bsgc->bsgp", bb_re, uf)
    bu_im = jnp.einsum("gpc,bsgc->bsgp", bb_im, uf)
    a_re = jnp.broadcast_to(ab_re, (1, s, SSM_GROUPS, SSM_STATE))
    a_im = jnp.broadcast_to(ab_im, (1, s, SSM_GROUPS, SSM_STATE))

    def combine(left, right):
        a1r, a1i, b1r, b1i = left
        a2r, a2i, b2r, b2i = right
        return (a1r * a2r - a1i * a2i, a1r * a2i + a1i * a2r,
                a2r * b1r - a2i * b1i + b2r, a2r * b1i + a2i * b1r + b2i)

    _, _, xr, xi = lax.associative_scan(combine, (a_re, a_im, bu_re, bu_im), axis=1)
    y = (jnp.einsum("gcp,bsgp->bsgc", c_re.astype(jnp.float32), xr)
         - jnp.einsum("gcp,bsgp->bsgc", c_im.astype(jnp.float32), xi))
    y = y.reshape(bsz, s, SSM_WIDTH) + d_skip.astype(jnp.float32) * u.astype(jnp.float32)
    return y.astype(u.dtype)


def hier_moe(h, w_rg, b_rg, w_re, b_re, w_gate, w_up, w_down):
    bsz, s, d = h.shape
    n_tok = bsz * s
    xt = h.reshape(n_tok, d)
    xf = xt.astype(jnp.float32)
    g_prob = jax.nn.softmax(xf @ w_rg.astype(jnp.float32) + b_rg.astype(jnp.float32), axis=-1)
    g_p, g_idx = lax.top_k(g_prob, 1)
    e_logits = (xf @ w_re.astype(jnp.float32) + b_re.astype(jnp.float32)).reshape(n_tok, N_GROUPS, EXPERTS_PER_GROUP)
    e_logits = e_logits[jnp.arange(n_tok), g_idx[:, 0]]
    e_prob = jax.nn.softmax(e_logits, axis=-1)
    e_p, e_local = lax.top_k(e_prob, EXPERT_TOPK)
    weights = g_p * e_p / jnp.sum(e_p, axis=-1, keepdims=True)
    experts = g_idx * EXPERTS_PER_GROUP + e_local
    n_assign = n_tok * EXPERT_TOPK
    flat_e = experts.reshape(-1)
    flat_w = weights.reshape(-1)
    flat_tok = jnp.arange(n_assign, dtype=jnp.int32) // EXPERT_TOPK
    order = jnp.argsort(flat_e)
    e_sorted = flat_e[order]
    counts = jnp.bincount(flat_e, length=N_EXPERTS)
    start = jnp.cumsum(counts) - counts
    padded = (counts + MOE_BLOCK - 1) // MOE_BLOCK * MOE_BLOCK
    pend = jnp.cumsum(padded)
    pstart = pend - padded
    dest = pstart[e_sorted] + (jnp.arange(n_assign) - start[e_sorted])
    n_blocks = -(-n_assign // MOE_BLOCK) + N_EXPERTS
    n_rows = n_blocks * MOE_BLOCK
    row_tok = jnp.full((n_rows,), n_tok, jnp.int32).at[dest].set(flat_tok[order])
    row_w = jnp.zeros((n_rows,), jnp.float32).at[dest].set(flat_w[order])
    block_e = jnp.minimum(jnp.searchsorted(pend, jnp.arange(n_blocks) * MOE_BLOCK, side="right"), N_EXPERTS - 1)
    x_pad = jnp.concatenate([xt, jnp.zeros((1, d), xt.dtype)], axis=0)
    x_rows = x_pad[row_tok].reshape(n_blocks, MOE_BLOCK, d)

    def expert_block(args):
        xb, e = args
        hid = jax.nn.silu(xb @ w_gate[e]) * (xb @ w_up[e])
        return hid @ w_down[e]

    y_rows = lax.map(expert_block, (x_rows, block_e)).reshape(n_rows, d)
    y = jax.ops.segment_sum(y_rows.astype(jnp.float32) * row_w[:, None], row_tok,
                            num_segments=n_tok + 1)[:n_tok]
    return y.reshape(bsz, s, d).astype(h.dtype)


def setup_inputs(seed: int = 0) -> dict:
    key = jax.random.key(seed)
    ks = iter(jax.random.split(key, 40))
    L = DEPTH

    def nrm(shape, scale):
        return jax.random.normal(next(ks), shape, jnp.float32) * scale

    n_idx = jnp.arange(SSM_STATE, dtype=jnp.float32)
    return {
        "x": nrm((BATCH, SEQ, D_MODEL), 1.0),
        "c": nrm((BATCH, D_MODEL), 1.0),
        "rel_bias": nrm((NUM_BUCKETS, ATT_HEADS), 0.5),
        "w_ada": nrm((L, D_MODEL, ADA_CHUNKS * D_MODEL), 0.2 * D_MODEL ** -0.5),
        "b_ada": nrm((L, ADA_CHUNKS * D_MODEL), 0.02),
        "g_pre_mix": 1.0 + nrm((L, D_MODEL), 0.05),
        "g_post_mix": 1.0 + nrm((L, D_MODEL), 0.05),
        "w_in": nrm((L, D_MODEL, IN_WIDTH), D_MODEL ** -0.5),
        "w_att_out": nrm((L, ATT_WIDTH, D_MODEL), ATT_WIDTH ** -0.5),
        "ssm_lambda_re": -0.5 + nrm((L, SSM_GROUPS, SSM_STATE), 0.01),
        "ssm_lambda_im": math.pi * n_idx + nrm((L, SSM_GROUPS, SSM_STATE), 0.01),
        "ssm_log_dt": jax.random.uniform(next(ks), (L, SSM_GROUPS), jnp.float32,
                                         minval=math.log(DT_MIN), maxval=math.log(DT_MAX)),
        "ssm_b_re": nrm((L, SSM_GROUPS, SSM_STATE, SSM_GROUP), (2 * SSM_GROUP) ** -0.5),
        "ssm_b_im": nrm((L, SSM_GROUPS, SSM_STATE, SSM_GROUP), (2 * SSM_GROUP) ** -0.5),
        "ssm_c_re": nrm((L, SSM_GROUPS, SSM_GROUP, SSM_STATE), (2 * SSM_STATE) ** -0.5),
        "ssm_c_im": nrm((L, SSM_GROUPS, SSM_GROUP, SSM_STATE), (2 * SSM_STATE) ** -0.5),
        "ssm_d": nrm((L, SSM_WIDTH), 1.0),
        "w_glu_val": nrm((L, SSM_WIDTH, D_MODEL), SSM_WIDTH ** -0.5),
        "w_glu_gate": nrm((L, SSM_WIDTH, D_MODEL), SSM_WIDTH ** -0.5),
        "w_mix_out": nrm((L, D_MODEL, D_MODEL), D_MODEL ** -0.5),
        "g_pre_ffn": 1.0 + nrm((L, D_MODEL), 0.05),
        "g_post_ffn": 1.0 + nrm((L, D_MODEL), 0.05),
        "w_router_group": nrm((L, D_MODEL, N_GROUPS), D_MODEL ** -0.5),
        "b_router_group": nrm((L, N_GROUPS), 0.01),
        "w_router_expert": nrm((L, D_MODEL, N_EXPERTS), D_MODEL ** -0.5),
        "b_router_expert": nrm((L, N_EXPERTS), 0.01),
        "w_exp_gate": nrm((L, N_EXPERTS, D_MODEL, D_EXPERT), D_MODEL ** -0.5),
        "w_exp_up": nrm((L, N_EXPERTS, D_MODEL, D_EXPERT), D_MODEL ** -0.5),
        "w_exp_down": nrm((L, N_EXPERTS, D_EXPERT, D_MODEL), D_EXPERT ** -0.5),
    }


def reference(x, c, rel_bias, w_ada, b_ada, g_pre_mix, g_post_mix, w_in, w_att_out,
              ssm_lambda_re, ssm_lambda_im, ssm_log_dt, ssm_b_re, ssm_b_im, ssm_c_re, ssm_c_im,
              ssm_d, w_glu_val, w_glu_gate, w_mix_out, g_pre_ffn, g_post_ffn,
              w_router_group, b_router_group, w_router_expert, b_router_expert,
              w_exp_gate, w_exp_up, w_exp_down):
    bsz, s, _ = x.shape
    split_points = [int(p) for p in np.cumsum(IN_SPLITS)[:-1]]
    c_act = jax.nn.silu(c)
    for l in range(DEPTH):
        mod = c_act @ w_ada[l] + b_ada[l]
        shift1, scale1, gate1, shift2, scale2, gate2 = [m[:, None, :] for m in jnp.split(mod, ADA_CHUNKS, axis=-1)]
        h = rms_norm(x, g_pre_mix[l]) * (1.0 + scale1) + shift1
        proj = h @ w_in[l]
        q, k, v, u, g_att, g_ssm = jnp.split(proj, split_points, axis=-1)
        q = q.reshape(bsz, s, ATT_HEADS, HEAD_DIM)
        k = k.reshape(bsz, s, ATT_HEADS, HEAD_DIM)
        v = v.reshape(bsz, s, ATT_HEADS, HEAD_DIM)
        a_br = moba_attention(q, k, v, rel_bias) @ w_att_out[l]
        y_ssm = s5_ssm(u, ssm_lambda_re[l], ssm_lambda_im[l], ssm_log_dt[l], ssm_b_re[l], ssm_b_im[l],
                       ssm_c_re[l], ssm_c_im[l], ssm_d[l])
        z = jax.nn.gelu(y_ssm)
        s_br = (z @ w_glu_val[l]) * jax.nn.sigmoid(z @ w_glu_gate[l])
        merged = jax.nn.sigmoid(g_att) * a_br + jax.nn.sigmoid(g_ssm) * s_br
        x = x + gate1 * rms_norm(merged @ w_mix_out[l], g_post_mix[l])
        h = rms_norm(x, g_pre_ffn[l]) * (1.0 + scale2) + shift2
        f = hier_moe(h, w_router_group[l], b_router_group[l], w_router_expert[l], b_router_expert[l],
                     w_exp_gate[l], w_exp_up[l], w_exp_down[l])
        x = x + gate2 * rms_norm(f, g_post_ffn[l])
    return x
```

```python
import math
from contextlib import ExitStack

import numpy as np
import concourse.bass as bass
import concourse.mybir as mybir
from concourse.bass_utils import run_bass_kernel_spmd

F32 = mybir.dt.float32
BF16 = mybir.dt.bfloat16
ALU = mybir.AluOpType
AF = mybir.ActivationFunctionType
AX = mybir.AxisListType

D = 1024
NCORES = 8
NEGM = -30000.0
BIG = 1.0e30


class Sched:
    def __init__(self, sems, dma_sems):
        self.ops = []
        self.last_w = {}
        self.readers = {}
        self.sems = sems
        self.dma_sems = dma_sems
        self.n_dma = 0
        self.waited = {}

    def add(self, eng, fn, reads=(), writes=(), dma=False):
        idx = len(self.ops)
        raw = set()
        other = set()
        for r in reads:
            if r in self.last_w:
                raw.add(self.last_w[r])
        for w in writes:
            if w in self.last_w:
                other.add(self.last_w[w])
            for rd in self.readers.get(w, ()):
                other.add(rd)
        for r in reads:
            self.readers.setdefault(r, []).append(idx)
        for w in writes:
            self.last_w[w] = idx
            self.readers[w] = []
        slot = None
        if dma:
            slot = self.n_dma % len(self.dma_sems)
            self.n_dma += 1
        self.ops.append(dict(eng=eng, fn=fn, raw=raw, other=other, dma=dma, slot=slot))
        return idx

    def mark(self, name):
        if not hasattr(self, 'marks'):
            self.marks = {}
        self.marks[name] = len(self.ops)

    def barrier(self, fns):
        first = []
        for e, fn in fns.items():
            first.append(self.add(e, fn, dma=(e == 'sp')))
        last_dma = {}
        for i, o in enumerate(self.ops):
            if o['dma']:
                last_dma[o['slot']] = i
        extra = set(first) | set(last_dma.values())
        for e, fn in fns.items():
            idx = self.add(e, fn, dma=(e == 'sp'))
            self.ops[idx]['raw'] |= extra

    def finalize(self):
        ops = self.ops
        for i, o in enumerate(ops):
            deps = set()
            for d in o['raw']:
                od = ops[d]
                if od['dma'] or od['eng'] != o['eng'] or o['eng'] != 'pe':
                    deps.add(d)
            for d in o['other']:
                od = ops[d]
                if od['dma'] or od['eng'] != o['eng']:
                    deps.add(d)
            deps.discard(i)
            o['deps'] = deps
        prev_slot_op = [None] * len(self.dma_sems)
        for i, o in enumerate(ops):
            if o['dma']:
                s = o['slot']
                if prev_slot_op[s] is not None:
                    o['deps'].add(prev_slot_op[s])
                prev_slot_op[s] = i
        needed = set()
        for o in ops:
            needed |= o['deps']
        cnt = {e: 0 for e in self.sems}
        dcnt = [0] * len(self.dma_sems)
        for i, o in enumerate(ops):
            if o['dma']:
                s = o['slot']
                dcnt[s] += 16
                o['tok'] = (('d', s), dcnt[s])
                o['signal'] = True
            elif i in needed:
                cnt[o['eng']] += 1
                o['tok'] = (('e', o['eng']), cnt[o['eng']])
                o['signal'] = True
            else:
                o['tok'] = None
                o['signal'] = False

    def _sem(self, k):
        return self.dma_sems[k[1]] if k[0] == 'd' else self.sems[k[1]]

    def emit(self, eng_name, eng):
        ops = self.ops
        waited = self.waited.setdefault(eng_name, {})
        for o in ops:
            if o['eng'] != eng_name:
                continue
            need = {}
            for d in o['deps']:
                k, v = ops[d]['tok']
                if waited.get(k, 0) >= v:
                    continue
                if need.get(k, 0) < v:
                    need[k] = v
            for k, v in need.items():
                eng.wait_ge(self._sem(k), v)
                waited[k] = v
            ins = o['fn'](eng)
            if o['signal']:
                ins.then_inc(self._sem(o['tok'][0]), 16 if o['dma'] else 1)

    def final_waits(self, eng):
        cnt = {}
        for o in self.ops:
            if o['tok'] is not None:
                k, v = o['tok']
                if cnt.get(k, 0) < v:
                    cnt[k] = v
        for k, v in cnt.items():
            eng.wait_ge(self._sem(k), v)


def _t5_bucket_np(d):
    n = np.maximum(d, 0)
    nf = np.maximum(n, 1).astype(np.float32)
    large = 16 + (np.log(nf / np.float32(16)) / np.float32(math.log(128 / 16)) * np.float32(16)).astype(np.int32)
    large = np.minimum(large, 31)
    return np.where(n < 16, n, large)


def _constants():
    c = {}
    dd = np.arange(-127, 257)
    bk = _t5_bucket_np(dd)
    oh = np.zeros((33, 384), np.float32)
    for j, d in enumerate(dd):
        if d < 0:
            oh[32, j] = 1.0
        else:
            oh[bk[j], j] = 1.0
    c["c_onehot"] = oh
    c["c_ident"] = np.eye(128, dtype=np.float32)
    s = np.arange(128)
    c["c_tri"] = (s[:, None] <= s[None, :]).astype(np.float32)
    e = np.zeros((16, 16, 128), np.float32)
    for n in range(16):
        e[n, n, :] = 1.0
    c["c_eall"] = e
    qb = np.arange(16)[:, None]
    n = np.arange(16)[None, :]
    c["c_past"] = (n < qb).astype(np.float32).reshape(1, 256)
    c["c_pneg"] = ((n < qb).astype(np.float32) - 1.0).reshape(1, 256) * np.float32(BIG)
    c["c_own"] = (n == qb).astype(np.float32).reshape(1, 256)
    c["c_tidx"] = np.tile(np.arange(128, dtype=np.float32)[None, :], (1, 1))
    return c


def _layout_weights(inp):
    w = {}
    f32 = np.float32
    w_ada = inp["w_ada"][0]
    w["w_ada"] = np.ascontiguousarray(w_ada.reshape(8, 128, 48, 128).transpose(2, 1, 0, 3))
    b_ada = inp["b_ada"][0]
    w["b_adaT"] = np.ascontiguousarray(b_ada.reshape(48, 128).T)
    w["b_ada_row"] = np.ascontiguousarray(b_ada.reshape(1, 6144))
    gT = np.stack([inp["g_pre_mix"][0].reshape(8, 128).T, inp["g_pre_ffn"][0].reshape(8, 128).T], axis=1)
    w["gpreT"] = np.ascontiguousarray(gT)
    w["gpost_rows"] = np.ascontiguousarray(np.stack([inp["g_post_mix"][0], inp["g_post_ffn"][0]])[None])
    w_in = inp["w_in"][0]
    w["w_in"] = np.ascontiguousarray(w_in.reshape(8, 128, 30, 128).transpose(2, 1, 0, 3))
    wao = inp["w_att_out"][0]
    w["w_ao"] = np.ascontiguousarray(wao.reshape(8, 64, 8, 128).transpose(2, 1, 0, 3))
    wgv = inp["w_glu_val"][0].reshape(2, 128, 8, 128).transpose(2, 1, 0, 3)
    wgg = inp["w_glu_gate"][0].reshape(2, 128, 8, 128).transpose(2, 1, 0, 3)
    w["w_glu"] = np.ascontiguousarray(np.stack([wgv, wgg], axis=2))
    w["w_mix"] = np.ascontiguousarray(inp["w_mix_out"][0].reshape(8, 128, 1024))
    lr = inp["ssm_lambda_re"][0]
    li = inp["ssm_lambda_im"][0]
    ld = inp["ssm_log_dt"][0]
    ldf = np.repeat(ld[:, None], 64, axis=1)

    def fm(a):
        return a.reshape(8, 2, 64).transpose(1, 2, 0).reshape(128, 8)
    w["ssmP"] = np.ascontiguousarray(np.stack([fm(lr), fm(li), fm(ldf)], axis=1))
    w["ssmF"] = np.ascontiguousarray(np.stack([lr.reshape(-1), li.reshape(-1), ldf.reshape(-1)])[None])
    br = inp["ssm_b_re"][0]
    bi = inp["ssm_b_im"][0]
    bbig = np.zeros((128, 2, 2, 512), f32)
    for g in range(16):
        hf, gl = g // 8, g % 8
        bbig[gl * 16:(gl + 1) * 16, hf, 0, gl * 64:(gl + 1) * 64] = br[g].T
        bbig[gl * 16:(gl + 1) * 16, hf, 1, gl * 64:(gl + 1) * 64] = bi[g].T
    w["bbig"] = bbig
    cr = inp["ssm_c_re"][0]
    ci = inp["ssm_c_im"][0]
    cpad = np.zeros((128, 2, 8, 128), f32)
    for g in range(16):
        gp, g2 = g // 2, g % 2
        gl = g % 8
        cpad[g2 * 64:(g2 + 1) * 64, 0, gp, gl * 16:(gl + 1) * 16] = cr[g].T
        cpad[g2 * 64:(g2 + 1) * 64, 1, gp, gl * 16:(gl + 1) * 16] = ci[g].T
    w["cpad"] = cpad
    w["ssm_dT"] = np.ascontiguousarray(inp["ssm_d"][0].reshape(2, 128).T)
    w["rel_bias"] = np.ascontiguousarray(inp["rel_bias"])
    w["rb31"] = np.ascontiguousarray(inp["rel_bias"][31:32, :])
    wr = np.concatenate([inp["w_router_group"][0], inp["w_router_expert"][0]], axis=1)
    w["w_rt"] = np.ascontiguousarray(wr.reshape(8, 128, 36).transpose(1, 0, 2))
    w["b_rt"] = np.ascontiguousarray(np.concatenate([inp["b_router_group"][0], inp["b_router_expert"][0]])[None])
    w["w_eg"] = np.ascontiguousarray(inp["w_exp_gate"][0].reshape(32, 8, 128, 512).transpose(0, 2, 1, 3))
    w["w_eu"] = np.ascontiguousarray(inp["w_exp_up"][0].reshape(32, 8, 128, 512).transpose(0, 2, 1, 3))
    w["w_ed"] = np.ascontiguousarray(inp["w_exp_down"][0].reshape(32, 4, 128, 1024).transpose(0, 2, 1, 3))
    return w


IN_SHAPES = {
    "w_ada": [48, 128, 8, 128], "b_adaT": [128, 48], "b_ada_row": [1, 6144], "gpreT": [128, 2, 8],
    "gpost_rows": [1, 2, 1024], "w_in": [30, 128, 8, 128], "w_ao": [8, 64, 8, 128], "w_glu": [8, 128, 2, 2, 128],
    "w_mix": [8, 128, 1024], "ssmP": [128, 3, 8], "ssmF": [1, 3, 1024], "bbig": [128, 2, 2, 512],
    "cpad": [128, 2, 8, 128], "ssm_dT": [128, 2], "rel_bias": [32, 8], "rb31": [1, 8],
    "w_rt": [128, 8, 36], "b_rt": [1, 36], "w_eg": [32, 128, 8, 512], "w_eu": [32, 128, 8, 512],
    "w_ed": [32, 128, 4, 1024],
    "c_onehot": [33, 384], "c_ident": [128, 128], "c_tri": [128, 128], "c_eall": [16, 16, 128],
    "c_past": [1, 256], "c_pneg": [1, 256], "c_own": [1, 256], "c_tidx": [1, 128],
}


def bc_rows(ap_row, nparts):
    pat = [list(x) for x in ap_row.ap]
    pat[0] = [0, nparts]
    return bass.AP(ap_row.tensor, ap_row.offset, pat)


def build(NSEQ, S, do_moe=True, dbg=False):
    GT = 256
    NG = S // GT
    NT = S // 128
    nc = bass.Bass("TRN2", target_bir_lowering=False)
    din = {}
    din["x"] = nc.dram_tensor("x", [NSEQ, S, D], F32, kind="ExternalInput").ap()
    din["cT"] = nc.dram_tensor("cT", [128, 8, NSEQ], F32, kind="ExternalInput").ap()
    for k, shp in IN_SHAPES.items():
        din[k] = nc.dram_tensor(k, shp, F32, kind="ExternalInput").ap()
    out = nc.dram_tensor("out", [NSEQ, S, D], F32, kind="ExternalOutput").ap()
    dbg_out = {}
    s_win = nc.dram_tensor("s_win", [30, 128, 1024], BF16).ap()
    s_wao = nc.dram_tensor("s_wao", [8, 64, 1024], BF16).ap()
    s_wglu = nc.dram_tensor("s_wglu", [8, 128, 512], BF16).ap()
    s_wmix = nc.dram_tensor("s_wmix", [8, 128, 1024], BF16).ap()
    s_weg = nc.dram_tensor("s_weg", [32, 128, 4096], BF16).ap()
    s_weu = nc.dram_tensor("s_weu", [32, 128, 4096], BF16).ap()
    s_wed = nc.dram_tensor("s_wed", [32, 128, 4096], BF16).ap()
    s_bias = nc.dram_tensor("s_bias", [8, 128, 384], F32).ap()

    es = ExitStack()
    with es:
        def sb(name, shape, dt):
            return es.enter_context(nc.sbuf_tensor("sb_" + name, shape, dt))

        def ps(name, shape, dt):
            return es.enter_context(nc.psum_tensor(name, shape, dt))

        sems = {e: es.enter_context(nc.semaphore("s_" + e)) for e in ['pe', 'act', 'dve', 'pool', 'sp']}
        dsems = [es.enter_context(nc.semaphore("d%d" % i)) for i in range(24)]
        SC = Sched(sems, dsems)
        A = SC.add

        pbank = [ps("pb%d" % i, [128, 512], F32) for i in range(8)]

        def pbf(i):
            return pbank[i]


        ident_b = sb("ident_b", [128, 128], BF16)
        ident_f = sb("ident_f", [128, 128], F32)
        tri_b = sb("tri_b", [128, 128], BF16)
        eall = sb("eall", [80, 16, 128], BF16)
        pastf = sb("pastf", [128, 16, 16], F32)
        pneg = sb("pneg", [128, 16, 16], F32)
        ownf = sb("ownf", [128, 16, 16], F32)
        cact = sb("cact", [128, 8, NSEQ], F32)
        badaT = sb("badaT", [128, 48], F32)
        gpreT = sb("gpreT", [128, 2, 8], F32)
        modT = sb("modT", [128, 4, 8, NSEQ], F32)
        ABt = sb("ABt", [128, 4, 8, NSEQ], F32)
        Grow = sb("Grow", [128, 1024], F32)
        ones_f = sb("ones_f", [128, 64], F32)
        ssmP = sb("ssmP", [128, 3, 8], F32)
        WpowR = sb("WpowR", [128, 8, 128], F32)
        WpowI = sb("WpowI", [128, 8, 128], F32)
        WinvR = sb("WinvR", [128, 1024], F32)
        WinvI = sb("WinvI", [128, 1024], F32)
        W128 = sb("W128", [128, 2, 8], F32)
        Bbig = sb("Bbig", [128, 2, 2, 512], BF16)
        Cterm = sb("Cterm", [128, 3, 8, 128], BF16)
        dT = sb("dT", [128, 2], F32)
        halfpi = sb("halfpi", [128, 1], F32)
        B01 = sb("B01", [128, 8, 256], F32)
        rb_a = sb("rb_a", [32, 8], F32)
        rb_b = sb("rb_b", [32, 8], F32)
        dmy = sb("dmy", [128, 8], F32)
        KT = sb("KT", [128, 4, S], BF16)
        Vt = sb("Vt", [128, NT, 8, 65], BF16)
        ksum = sb("ksum", [128, 4, 16], F32)
        kmeanT = sb("kmeanT", [128, 4, 16], BF16)
        XT = [sb("XT%d" % i, [128, 1024], F32) for i in range(3)]
        xs_b = [sb("xs_b%d" % i, [128, 1024], BF16) for i in range(2)]
        hT = sb("hT", [128, 8, GT], BF16)
        WS = [sb("WS%d" % i, [128, 1024], BF16) for i in range(6)]
        WSF = [sb("WSF%d" % i, [128, 1024], F32) for i in range(3)]
        QA = sb("QA", [128, 4, GT], BF16)
        MT = sb("MT", [80, 4, GT], BF16)
        Gs = sb("Gs", [128, 8, 16], F32)
        G2 = sb("G2", [128, 8, 16], F32)
        Eq = sb("Eq", [128, 8, 16], F32)
        mx = sb("mx", [128, 3, 8], F32)
        Msb = sb("Msb", [128, 8, 16], BF16)
        sbias = [sb("sbias%d" % i, [128, GT], F32) for i in range(2)]
        PT = [sb("PT%d" % i, [128, GT], BF16) for i in range(3)]
        OT = sb("OT", [64, 8, GT], BF16)
        rden = sb("rden", [65, GT], F32)
        rbc = sb("rbc", [64, GT], F32)
        uT = sb("uT", [128, 2, GT], F32)
        uTb = sb("uTb", [128, 2, GT], BF16)
        Uq = [sb("Uq%d" % i, [128, 2, 512], BF16) for i in range(4)]
        TP = sb("TP", [128, 4, 8, 128], BF16)
        car = [sb("car%d" % i, [128, 2, 8], F32) for i in range(2)]
        ctmp = sb("ctmp", [128, 6, 4], F32)
        ysb = sb("ysb", [128, 128], F32)
        ysq = sb("ysq", [128, 128], F32)
        zT = sb("zT", [128, 2, GT], BF16)
        sgA = [sb("sgA%d" % i, [128, GT], F32) for i in range(3)]
        mt1 = sb("mt1", [128, GT], F32)
        mt2 = sb("mt2", [128, GT], F32)
        mergedT = sb("mergedT", [128, 8, GT], BF16)
        stat = sb("stat", [128, 8], F32)
        rtmp = sb("rtmp", [128, 1024], F32)

        tA = []
        for t_ in (XT[0], XT[1], XT[2], rtmp):
            tA.append(t_[:, 0:512])
            tA.append(t_[:, 512:1024])
        TPf = TP[:].rearrange("p a b c -> p (a b c)").bitcast(F32)
        FRh = TPf[:, 0:1536].rearrange("p (a b) -> p a b", a=3)
        Bf_re = Uq[0][:].rearrange("p a b -> p (a b)").bitcast(F32)
        Bf_im = Uq[1][:].rearrange("p a b -> p (a b)").bitcast(F32)
        Cf_a = Uq[2][:].rearrange("p a b -> p (a b)").bitcast(F32)
        Cf_b = Uq[3][:].rearrange("p a b -> p (a b)").bitcast(F32)
        rbl = mergedT[:].rearrange("p a b -> p (a b)").bitcast(F32)[0:33, :].rearrange("p (a b) -> p a b", a=8)
        oneh = hT[:].rearrange("p a b -> p (a b)").bitcast(F32)[0:33, 0:384]

        def ld(eng, dst, src, wname, reads=()):
            A(eng, lambda e, dst=dst, src=src: e.dma_start(out=dst, in_=src), reads=list(reads), writes=[wname], dma=True)

        ld('sp', ident_f[:], din["c_ident"], 'ident_f')
        ld('pool', ident_b[:], din["c_ident"], 'ident_b')
        ld('pool', tri_b[:], din["c_tri"], 'tri_b')
        ld('pool', eall[0:16], din["c_eall"], 'eall')
        ld('pool', eall[64:80], din["c_eall"], 'eall')
        ld('sp', pastf[:].rearrange("p a b -> p (a b)"), bc_rows(din["c_past"], 128), 'pastf')
        ld('sp', pneg[:].rearrange("p a b -> p (a b)"), bc_rows(din["c_pneg"], 128), 'pneg')
        ld('sp', ownf[:].rearrange("p a b -> p (a b)"), bc_rows(din["c_own"], 128), 'ownf')
        ld('sp', cact[:], din["cT"], 'cact')
        ld('sp', badaT[:], din["b_adaT"], 'badaT')
        ld('sp', gpreT[:], din["gpreT"], 'gpreT')
        ld('sp', ssmP[:], din["ssmP"], 'ssmP')
        ld('sp', dT[:], din["ssm_dT"], 'dT')
        A('dve', lambda e: e.memset(ones_f[:], 1.0), writes=['ones_f'])
        A('dve', lambda e: e.memset(halfpi[:], math.pi / 2), writes=['halfpi'])
        A('dve', lambda e: e.memset(dmy[:], 0.0), writes=['dmy'])
        A('act', lambda e: e.activation(out=cact[:], in_=cact[:], func=AF.Silu), reads=['cact'], writes=['cact'])

        SC.mark('a_casts')
        WAv = [(rtmp[:].rearrange("p (a b) -> p a b", a=8), 'rtmp'), (XT[0][:].rearrange("p (a b) -> p a b", a=8), ('XT', 0))]
        CBv, CBtag = XT[2][:].rearrange("p (a b) -> p a b", a=8), ('XT', 2)
        rowtmp, rowtag = XT[1], ('XT', 1)
        wa_cnt = [0]

        def load_wa(pc):
            k = wa_cnt[0] % 2
            wa_cnt[0] += 1
            ld('sp', WAv[k][0], din["w_ada"][pc], WAv[k][1])
            return k

        fm_js = [0, 1, 3, 4]
        for mi, j in enumerate(fm_js):
            for fc in range(8):
                k = load_wa(j * 8 + fc)
                for kc in range(8):
                    A('pe', lambda e, k=k, kc=kc: e.matmul(pbank[0][:, 0:NSEQ], lhsT=WAv[k][0][:, kc, :], rhs=cact[:, kc, :], start=(kc == 0), stop=(kc == 7)),
                      reads=[WAv[k][1], 'cact'], writes=[('ps', 0)])
                A('act', lambda e, mi=mi, fc=fc, j=j: e.activation(out=modT[:, mi, fc, :], in_=pbank[0][:, 0:NSEQ], func=AF.Identity, bias=badaT[:, j * 8 + fc:j * 8 + fc + 1]),
                  reads=[('ps', 0), 'badaT'], writes=['modT'])
        for (ai, si, hi, gi) in [(0, 1, 0, 0), (2, 3, 2, 1)]:
            A('dve', lambda e, ai=ai, si=si, gi=gi: e.scalar_tensor_tensor(out=ABt[:, ai], in0=modT[:, si], scalar=1.0, in1=gpreT[:, gi, :].unsqueeze(2).to_broadcast([128, 8, NSEQ]), op0=ALU.add, op1=ALU.mult),
              reads=['modT', 'gpreT'], writes=['ABt'])
            A('dve', lambda e, ai=ai, hi=hi: e.tensor_copy(out=ABt[:, ai + 1], in_=modT[:, hi]), reads=['modT'], writes=['ABt'])

        def gate_rows(j, b, gidx):
            A('dve', lambda e, b=b: e.tensor_copy(out=CBv, in_=cact[:, :, b:b + 1].to_broadcast([128, 8, 128])), reads=['cact'], writes=[CBtag])
            ld('sp', rowtmp[:], bc_rows(din["b_ada_row"][:, j * 1024:(j + 1) * 1024], 128), rowtag)
            for pcl in range(8):
                k = load_wa(j * 8 + pcl)
                for kc in range(8):
                    A('pe', lambda e, k=k, kc=kc: e.matmul(pbank[0][:, 0:128], lhsT=CBv[:, kc, :], rhs=WAv[k][0][:, kc, :], start=(kc == 0), stop=(kc == 7)),
                      reads=[WAv[k][1], CBtag], writes=[('ps', 0)])
                A('dve', lambda e, pcl=pcl: e.tensor_tensor(out=Grow[:, pcl * 128:(pcl + 1) * 128], in0=pbank[0][:, 0:128], in1=rowtmp[:, pcl * 128:(pcl + 1) * 128], op=ALU.add),
                  reads=[('ps', 0), rowtag], writes=['Grow'])
            ld('sp', rowtmp[:], bc_rows(din["gpost_rows"][:, gidx, :], 128), rowtag, reads=['Grow'])
            A('dve', lambda e: e.tensor_tensor(out=Grow[:], in0=Grow[:], in1=rowtmp[:], op=ALU.mult), reads=['Grow', rowtag], writes=['Grow'])

        SC.mark('b_adaln')
        def abar_small(lr, li, ldt, n, outs, sign, tg):
            t_dt, t_m, t_s, t_c = [tA[4 + i][:, 0:n] for i in range(4)]
            o_re, o_im = outs
            A('act', lambda e: e.activation(out=t_dt, in_=ldt, func=AF.Exp), reads=[tg, 'abar_o'], writes=['tA4'])
            A('dve', lambda e: e.tensor_tensor(out=t_m, in0=lr, in1=t_dt, op=ALU.mult), reads=['tA4', tg], writes=['tA5'])
            A('act', lambda e: e.activation(out=t_m, in_=t_m, func=AF.Exp, scale=sign / 32.0), reads=['tA5'], writes=['tA5'])
            A('dve', lambda e: e.tensor_tensor(out=t_s, in0=li, in1=t_dt, op=ALU.mult), reads=['tA4', tg], writes=['tA6'])
            A('act', lambda e: e.activation(out=t_c, in_=t_s, func=AF.Sin, scale=sign / 32.0, bias=halfpi[:]), reads=['tA6', 'halfpi'], writes=['tA7'])
            A('act', lambda e: e.activation(out=t_s, in_=t_s, func=AF.Sin, scale=sign / 32.0), reads=['tA6'], writes=['tA6'])
            A('dve', lambda e: e.tensor_tensor(out=o_re, in0=t_m, in1=t_c, op=ALU.mult), reads=['tA5', 'tA7'], writes=['abar_o'])
            A('dve', lambda e: e.tensor_tensor(out=o_im, in0=t_m, in1=t_s, op=ALU.mult), reads=['tA5', 'tA6'], writes=['abar_o'])
            for _ in range(5):
                A('dve', lambda e: e.tensor_tensor(out=t_c, in0=o_re, in1=o_re, op=ALU.mult), reads=['abar_o'], writes=['tA7'])
                A('dve', lambda e: e.tensor_tensor(out=t_s, in0=o_im, in1=o_im, op=ALU.mult), reads=['abar_o'], writes=['tA6'])
                A('dve', lambda e: e.tensor_tensor(out=t_m, in0=o_re, in1=o_im, op=ALU.mult), reads=['abar_o'], writes=['tA5'])
                A('dve', lambda e: e.tensor_tensor(out=o_re, in0=t_c, in1=t_s, op=ALU.subtract), reads=['tA7', 'tA6'], writes=['abar_o'])
                A('dve', lambda e: e.tensor_scalar(out=o_im, in0=t_m, scalar1=2.0, scalar2=None, op0=ALU.mult), reads=['tA5'], writes=['abar_o'])

        def cmul_bc(o_re, o_im, a_re, a_im, b_re, b_im, tg_r, tg_w, tmp0, tmp1):
            A('dve', lambda e: e.tensor_tensor(out=tmp0, in0=a_re, in1=b_re, op=ALU.mult), reads=tg_r, writes=['cm0'])
            A('dve', lambda e: e.tensor_tensor(out=tmp1, in0=a_im, in1=b_im, op=ALU.mult), reads=tg_r, writes=['cm1'])
            A('dve', lambda e: e.tensor_tensor(out=o_re, in0=tmp0, in1=tmp1, op=ALU.subtract), reads=['cm0', 'cm1'], writes=tg_w)
            A('dve', lambda e: e.tensor_tensor(out=tmp0, in0=a_re, in1=b_im, op=ALU.mult), reads=tg_r + tg_w, writes=['cm0'])
            A('dve', lambda e: e.tensor_tensor(out=tmp1, in0=a_im, in1=b_re, op=ALU.mult), reads=tg_r + tg_w, writes=['cm1'])
            A('dve', lambda e: e.tensor_tensor(out=o_im, in0=tmp0, in1=tmp1, op=ALU.add), reads=['cm0', 'cm1'], writes=tg_w)

        def power_table(TR, TI, sign):
            ab_re = tA[0][:, 0:8]
            ab_im = tA[0][:, 8:16]
            abar_small(ssmP[:, 0, :], ssmP[:, 1, :], ssmP[:, 2, :], 8, (ab_re, ab_im), sign, 'ssmP')
            A('dve', lambda e: e.memset(TR[:, :, 0:1], 1.0), writes=['ptab'])
            A('dve', lambda e: e.memset(TI[:, :, 0:1], 0.0), writes=['ptab'])
            pw_re = tA[0][:, 16:24]
            pw_im = tA[0][:, 24:32]
            A('dve', lambda e: e.tensor_copy(out=pw_re, in_=ab_re), reads=['abar_o'], writes=['pw'])
            A('dve', lambda e: e.tensor_copy(out=pw_im, in_=ab_im), reads=['abar_o'], writes=['pw'])
            n = 1
            while n < 128:
                shp = [128, 8, n]
                cmul_bc(TR[:, :, n:2 * n], TI[:, :, n:2 * n], TR[:, :, 0:n], TI[:, :, 0:n],
                        pw_re.unsqueeze(2).to_broadcast(shp), pw_im.unsqueeze(2).to_broadcast(shp),
                        ['ptab', 'pw'], ['ptab'],
                        tA[1][:, 0:8 * n].rearrange("p (a b) -> p a b", a=8), tA[2][:, 0:8 * n].rearrange("p (a b) -> p a b", a=8))
                n *= 2
                if n < 128 or sign > 0:
                    cmul_bc(tA[0][:, 32:40], tA[0][:, 40:48], pw_re, pw_im, pw_re, pw_im, ['pw'], ['pw2'], tA[1][:, 0:8], tA[2][:, 0:8])
                    A('dve', lambda e: e.tensor_copy(out=tA[0][:, 16:32], in_=tA[0][:, 32:48]), reads=['pw2'], writes=['pw'])
            return pw_re, pw_im

        pw_re, pw_im = power_table(WpowR, WpowI, 1.0)
        A('dve', lambda e: e.tensor_copy(out=W128[:, 0, :], in_=pw_re), reads=['pw'], writes=['W128'])
        A('dve', lambda e: e.tensor_copy(out=W128[:, 1, :], in_=pw_im), reads=['pw'], writes=['W128'])
        InvR = WinvR[:].rearrange("p (a b) -> p a b", a=8)
        InvI = WinvI[:].rearrange("p (a b) -> p a b", a=8)
        power_table(InvR, InvI, -1.0)
        for T_ in (WinvR, WinvI):
            for q4 in range(2):
                for gl in range(4):
                    gp = q4 * 4 + gl
                    A('pe', lambda e, T_=T_, gp=gp, gl=gl: e.transpose(out=pbank[1][:, gl * 128:(gl + 1) * 128], in_=T_[:, gp * 128:(gp + 1) * 128], identity=ident_f[:]),
                      reads=['ptab', 'ident_f'], writes=[('ps', 1)])
                A('act', lambda e, q4=q4: e.activation(out=tA[3 + q4], in_=pbank[1][:, :], func=AF.Copy), reads=[('ps', 1), 'abar_o'], writes=['tAtr'])
            A('dve', lambda e, T_=T_: e.tensor_copy(out=T_[:, 0:512], in_=tA[3]), reads=['tAtr', 'ptab'], writes=['ptab'])
            A('dve', lambda e, T_=T_: e.tensor_copy(out=T_[:, 512:1024], in_=tA[4]), reads=['tAtr', 'ptab'], writes=['ptab'])
        for hf in range(2):
            ld('sp', FRh, bc_rows(din["ssmF"][:, :, hf * 512:(hf + 1) * 512], 128), 'FR', reads=['Bbig'])
            ld('sp', Bf_re, din["bbig"][:, hf, 0, :], 'Bf', reads=['Bbig'])
            ld('sp', Bf_im, din["bbig"][:, hf, 1, :], 'Bf', reads=['Bbig'])
            abF_re, abF_im = tA[0], tA[1]
            abar_small(FRh[:, 0, :], FRh[:, 1, :], FRh[:, 2, :], 512, (abF_re, abF_im), 1.0, 'FR')
            den, fre, fim, t7 = tA[4], tA[5], tA[6], tA[7]
            lrF, liF = FRh[:, 0, :], FRh[:, 1, :]
            A('dve', lambda e: e.tensor_tensor(out=den, in0=lrF, in1=lrF, op=ALU.mult), reads=['FR', 'abar_o'], writes=['tA4'])
            A('dve', lambda e: e.tensor_tensor(out=t7, in0=liF, in1=liF, op=ALU.mult), reads=['FR', 'abar_o'], writes=['tA7'])
            A('dve', lambda e: e.tensor_tensor(out=den, in0=den, in1=t7, op=ALU.add), reads=['tA4', 'tA7'], writes=['tA4'])
            A('dve', lambda e: e.reciprocal(out=den, in_=den), reads=['tA4'], writes=['tA4'])
            A('dve', lambda e: e.tensor_scalar(out=abF_re, in0=abF_re, scalar1=-1.0, scalar2=None, op0=ALU.add), reads=['abar_o'], writes=['abar_o'])
            A('dve', lambda e: e.tensor_tensor(out=fre, in0=abF_re, in1=lrF, op=ALU.mult), reads=['abar_o', 'FR'], writes=['tA5'])
            A('dve', lambda e: e.tensor_tensor(out=t7, in0=abF_im, in1=liF, op=ALU.mult), reads=['abar_o', 'FR', 'tA4'], writes=['tA7'])
            A('dve', lambda e: e.tensor_tensor(out=fre, in0=fre, in1=t7, op=ALU.add), reads=['tA5', 'tA7'], writes=['tA5'])
            A('dve', lambda e: e.tensor_tensor(out=fre, in0=fre, in1=den, op=ALU.mult), reads=['tA5', 'tA4'], writes=['tA5'])
            A('dve', lambda e: e.tensor_tensor(out=fim, in0=abF_im, in1=lrF, op=ALU.mult), reads=['abar_o', 'FR'], writes=['tA6'])
            A('dve', lambda e: e.tensor_tensor(out=t7, in0=abF_re, in1=liF, op=ALU.mult), reads=['abar_o', 'FR', 'tA5'], writes=['tA7'])
            A('dve', lambda e: e.tensor_tensor(out=fim, in0=fim, in1=t7, op=ALU.subtract), reads=['tA6', 'tA7'], writes=['tA6'])
            A('dve', lambda e: e.tensor_tensor(out=fim, in0=fim, in1=den, op=ALU.mult), reads=['tA6', 'tA4'], writes=['tA6'])
            bq0, bq1 = tA[2], tA[3]
            A('dve', lambda e: e.tensor_tensor(out=bq0, in0=Bf_re, in1=fre, op=ALU.mult), reads=['Bf', 'tA5', 'tAtr', 'ptab'], writes=['bt0'])
            A('dve', lambda e: e.tensor_tensor(out=bq1, in0=Bf_im, in1=fim, op=ALU.mult), reads=['Bf', 'tA6', 'tAtr', 'ptab'], writes=['bt1'])
            A('dve', lambda e, hf=hf: e.tensor_tensor(out=Bbig[:, hf, 0, :], in0=bq0, in1=bq1, op=ALU.subtract), reads=['bt0', 'bt1'], writes=['Bbig'])
            A('dve', lambda e: e.tensor_tensor(out=bq0, in0=Bf_im, in1=fre, op=ALU.mult), reads=['Bf', 'tA5', 'Bbig'], writes=['bt0'])
            A('dve', lambda e: e.tensor_tensor(out=bq1, in0=Bf_re, in1=fim, op=ALU.mult), reads=['Bf', 'tA6', 'Bbig'], writes=['bt1'])
            A('dve', lambda e, hf=hf: e.tensor_tensor(out=Bbig[:, hf, 1, :], in0=bq0, in1=bq1, op=ALU.add), reads=['bt0', 'bt1'], writes=['Bbig'])
        for ri in range(2):
            for hq in range(2):
                cf = Cf_a if hq == 0 else Cf_b
                src = din["cpad"][:, ri, hq * 4:(hq + 1) * 4, :].rearrange("p a b -> p (a b)")
                ld('sp', cf, src, ('Cf', hq), reads=['Bbig'])
                cf3 = cf.rearrange("p (a b) -> p a b", a=4)
                gs = slice(hq * 4, hq * 4 + 4)
                if ri == 0:
                    A('act', lambda e, cf3=cf3, gs=gs: e.activation(out=Cterm[:, 0, gs, :], in_=cf3, func=AF.Copy), reads=[('Cf', hq)], writes=['Cterm'])
                    A('act', lambda e, cf3=cf3, gs=gs: e.activation(out=Cterm[:, 1, gs, :], in_=cf3, func=AF.Copy, scale=-1.0), reads=[('Cf', hq)], writes=['Cterm'])
                else:
                    A('act', lambda e, cf3=cf3, gs=gs: e.activation(out=Cterm[:, 2, gs, :], in_=cf3, func=AF.Copy, scale=-1.0), reads=[('Cf', hq)], writes=['Cterm'])

        SC.mark('c_ssmtab')
        ld('sp', rb_a[:], din["rel_bias"], 'rb_a')
        ld('sp', rb_b[:], bc_rows(din["rb31"], 32), 'rb_b')
        ld('sp', oneh, din["c_onehot"], 'oneh')
        A('dve', lambda e: e.tensor_tensor(out=rb_a[:], in0=rb_a[:], in1=rb_b[:], op=ALU.subtract), reads=['rb_a', 'rb_b'], writes=['rb_a'])
        A('dve', lambda e: e.memset(rbl[32:33, :, :], NEGM), writes=['rbl'])
        A('dve', lambda e: e.tensor_copy(out=rbl[0:32, :, :], in_=rb_a[:].unsqueeze(2).to_broadcast([32, 8, 128])), reads=['rb_a'], writes=['rbl'])
        btmp = xs_b[0][:].bitcast(F32)
        for h in range(8):
            A('pe', lambda e, h=h: e.matmul(pbank[2][:, 0:384], lhsT=rbl[:, h, :], rhs=oneh, start=True, stop=True), reads=['rbl', 'oneh'], writes=[('ps', 2)])
            A('act', lambda e: e.activation(out=btmp[:, 0:384], in_=pbank[2][:, 0:384], func=AF.Copy), reads=[('ps', 2)], writes=['btmp'])
            A('pool', lambda e, h=h: e.dma_start(out=s_bias[h], in_=btmp[:, 0:384]), reads=['btmp'], writes=[('s_bias', h)], dma=True)
            toe = bass.AP(s_bias.tensor, h * 128 * 384 + 127, [[383, 128], [1, 256]])
            A('sp', lambda e, h=h, toe=toe: e.dma_start(out=B01[:, h, :], in_=toe), reads=[('s_bias', h)], writes=['B01'], dma=True)

        A('pool', lambda e: e.memset(kmeanT[:], 0.0), writes=['kmeanT'])
        A('pool', lambda e: e.memset(Vt[:, :, :, 64:65], 1.0), writes=['Vones'])

        SC.mark('d_bias')
        bar_fns = {
            'pe': lambda e: e.matmul(pbank[7][0:1, 0:1], lhsT=ident_f[0:1, 0:1], rhs=ident_f[0:1, 0:1], start=True, stop=True),
            'act': lambda e: e.activation(out=dmy[:, 0:1], in_=dmy[:, 1:2], func=AF.Copy),
            'dve': lambda e: e.memset(dmy[:, 2:3], 0.0),
            'pool': lambda e: e.memset(dmy[:, 3:4], 0.0),
            'sp': lambda e: e.dma_start(out=dmy[0:1, 4:5], in_=din["c_tidx"][0:1, 0:1]),
        }
        SC.barrier(bar_fns)

        ws_cnt = [0]

        def wload(src, view):
            k = ws_cnt[0] % len(WS)
            kf = ws_cnt[0] % len(WSF)
            ws_cnt[0] += 1
            npart = src.shape[0]
            nfree = int(np.prod(src.shape[1:]))
            A('sp', lambda e, kf=kf, src=src, npart=npart, nfree=nfree: e.dma_start(out=WSF[kf][0:npart, 0:nfree], in_=src), writes=[('WSF', kf)], dma=True)
            A('pool', lambda e, k=k, kf=kf, npart=npart, nfree=nfree: e.tensor_copy(out=WS[k][0:npart, 0:nfree], in_=WSF[kf][0:npart, 0:nfree]), reads=[('WSF', kf)], writes=[('WS', k)])
            return k

        def rms_stats(eng_sq, src_ap, col, res_reads):
            A('dve', lambda e: e.memset(stat[:, col:col + 1], 0.0), writes=[('stat', col)])
            A('act', lambda e: e.activation(out=rtmp[:], in_=src_ap, func=AF.Square, accum_out=stat[:, col:col + 1]), reads=list(res_reads) + [('stat', col)], writes=['rtmp', ('stat', col)])
            A('dve', lambda e: e.tensor_scalar(out=stat[:, col:col + 1], in0=stat[:, col:col + 1], scalar1=1.0 / 1024, scalar2=1e-6, op0=ALU.mult, op1=ALU.add), reads=[('stat', col)], writes=[('stat', col)])
            A('act', lambda e: e.activation(out=stat[:, col:col + 1], in_=stat[:, col:col + 1], func=AF.Sqrt), reads=[('stat', col)], writes=[('stat', col)])
            A('dve', lambda e: e.reciprocal(out=stat[:, col:col + 1], in_=stat[:, col:col + 1]), reads=[('stat', col)], writes=[('stat', col)])

        pT_b = pbank[0][:].bitcast(BF16).rearrange("p (a b) -> p a b", a=8)
        pM_b = pbank[1][:].bitcast(BF16)
        pM2_b = pbank[2][:].bitcast(BF16)

        xt_cnt = [0]
        for b in range(NSEQ):
            gate_rows(2, b, 0)
            A('dve', lambda e: e.memset(ksum[:], 0.0), reads=['kmeanT'], writes=['ksum'])
            for cc in range(2):
                A('dve', lambda e, cc=cc: e.memset(car[cc][:], 0.0), writes=[('car', cc)])
            ch_idx = 0
            for g in range(NG):
                t0 = g * GT
                xslots = []
                for j in range(2):
                    xk = xt_cnt[0] % 3
                    xt_cnt[0] += 1
                    xslots.append(xk)
                    ld('sp', XT[xk][:], din["x"][b, t0 + j * 128:t0 + (j + 1) * 128, :], ('XT', xk))
                    rms_stats('act', XT[xk][:], j, [('XT', xk)])
                    A('act', lambda e, xk=xk, j=j: e.activation(out=xs_b[j][:], in_=XT[xk][:], func=AF.Copy, scale=stat[:, j:j + 1]), reads=[('XT', xk), ('stat', j)], writes=[('xs_b', j)])
                    for fc in range(8):
                        A('pe', lambda e, j=j, fc=fc: e.transpose(out=pT_b[:, fc, :], in_=xs_b[j][:, fc * 128:(fc + 1) * 128], identity=ident_b[:]), reads=[('xs_b', j), 'ident_b'], writes=[('ps', 0)])
                    for fc in range(8):
                        A('act', lambda e, j=j, fc=fc, b=b: e.activation(out=hT[:, fc, j * 128:(j + 1) * 128], in_=pT_b[:, fc, :], func=AF.Identity, scale=ABt[:, 0, fc, b:b + 1], bias=ABt[:, 1, fc, b:b + 1]),
                          reads=[('ps', 0), 'ABt'], writes=['hT'])
                SC.mark('g%d_%d_a_norm' % (b, g))
                pcnt = [0]

                def proj_fm(oc, bank):
                    k = wload(din["w_in"][oc].rearrange("p a b -> p (a b)"), [])
                    wv = WS[k][:, :].rearrange("p (a b) -> p a b", a=8)
                    for kc in range(8):
                        A('pe', lambda e, kc=kc, wv=wv, bank=bank: e.matmul(pbank[bank][:, 0:GT], lhsT=wv[:, kc, :], rhs=hT[:, kc, :], start=(kc == 0), stop=(kc == 7)),
                          reads=[('WS', k), 'hT'], writes=[('ps', bank)])

                for hp in range(4):
                    bank = 1 + (pcnt[0] % 2); pcnt[0] += 1
                    proj_fm(hp, bank)
                    A('act', lambda e, hp=hp, bank=bank: e.activation(out=QA[:, hp, :], in_=pbank[bank][:, 0:GT], func=AF.Copy, scale=0.125), reads=[('ps', bank)], writes=['QA'])
                for hp in range(4):
                    bank = 1 + (pcnt[0] % 2); pcnt[0] += 1
                    proj_fm(4 + hp, bank)
                    A('act', lambda e, hp=hp, bank=bank, g=g, t0=t0: e.activation(out=KT[:, hp, t0:t0 + GT], in_=pbank[bank][:, 0:GT], func=AF.Copy, accum_out=ksum[:, hp, g:g + 1]),
                      reads=[('ps', bank)], writes=[('KT', g), 'ksum'])
                for hp in range(4):
                    k = wload(din["w_in"][8 + hp].rearrange("p a b -> p (a b)"), [])
                    wv = WS[k][:, :].rearrange("p (a b) -> p a b", a=8)
                    for j in range(2):
                        bank = 1 + (pcnt[0] % 2); pcnt[0] += 1
                        for kc in range(8):
                            A('pe', lambda e, kc=kc, wv=wv, bank=bank, j=j: e.matmul(pbank[bank][:, 0:128], lhsT=hT[:, kc, j * 128:(j + 1) * 128], rhs=wv[:, kc, :], start=(kc == 0), stop=(kc == 7)),
                              reads=[('WS', k), 'hT'], writes=[('ps', bank)])
                        A('dve', lambda e, bank=bank, j=j, hp=hp, g=g: e.tensor_copy(out=Vt[:, 2 * g + j, 2 * hp:2 * hp + 2, 0:64], in_=pbank[bank][:, 0:128].rearrange("p (a b) -> p a b", a=2)),
                          reads=[('ps', bank)], writes=[('Vt', g)])
                for hf in range(2):
                    bank = 1 + (pcnt[0] % 2); pcnt[0] += 1
                    proj_fm(12 + hf, bank)
                    A('act', lambda e, hf=hf, bank=bank: e.activation(out=uT[:, hf, :], in_=pbank[bank][:, 0:GT], func=AF.Copy), reads=[('ps', bank)], writes=['uT'])
                    A('pool', lambda e, hf=hf: e.tensor_copy(out=uTb[:, hf, :], in_=uT[:, hf, :]), reads=['uT'], writes=['uTb'])

                SC.mark('g%d_%d_b_proj' % (b, g))
                for j in range(2):
                    pgs = [pbank[3 + par][:, 0:64].rearrange("p (a b) -> p a b", a=4) for par in range(2)]
                    for h in range(8):
                        hb, hp, par = (h % 2) * 64, h // 2, h % 2
                        A('pe', lambda e, hb=hb, hp=hp, j=j, pgv=pgs[par]: e.matmul(pgv[:, hp, :], lhsT=QA[hb:hb + 64, hp, j * 128:(j + 1) * 128], rhs=kmeanT[hb:hb + 64, hp, :], start=True, stop=True),
                          reads=['QA', 'kmeanT'], writes=[('ps', 3 + par)])
                    bshape = [128, 8, 16]
                    for par in range(2):
                        A('dve', lambda e, g=g, par=par, pgv=pgs[par]: e.tensor_tensor(out=Gs[:, par * 4:(par + 1) * 4, :], in0=pgv, in1=pneg[:, g, :].unsqueeze(1).to_broadcast([128, 4, 16]), op=ALU.add), reads=[('ps', 3 + par), 'pneg'], writes=['Gs'])
                    A('dve', lambda e: e.tensor_reduce(out=mx[:, 0, :], in_=Gs[:], axis=AX.X, op=ALU.max), reads=['Gs'], writes=['mx0'])
                    A('dve', lambda e: e.tensor_tensor(out=Eq[:], in0=Gs[:], in1=mx[:, 0, :].unsqueeze(2).to_broadcast(bshape), op=ALU.is_ge), reads=['Gs', 'mx0'], writes=['Eq'])
                    A('dve', lambda e: e.scalar_tensor_tensor(out=G2[:], in0=Eq[:], scalar=-BIG, in1=Gs[:], op0=ALU.mult, op1=ALU.add), reads=['Eq', 'Gs'], writes=['G2'])
                    A('dve', lambda e: e.tensor_reduce(out=mx[:, 1, :], in_=G2[:], axis=AX.X, op=ALU.max), reads=['G2'], writes=['mx1'])
                    A('dve', lambda e: e.tensor_tensor(out=Eq[:], in0=G2[:], in1=mx[:, 1, :].unsqueeze(2).to_broadcast(bshape), op=ALU.is_ge), reads=['G2', 'mx1'], writes=['Eq'])
                    A('dve', lambda e: e.scalar_tensor_tensor(out=G2[:], in0=Eq[:], scalar=-BIG, in1=G2[:], op0=ALU.mult, op1=ALU.add), reads=['Eq', 'G2'], writes=['G2'])
                    A('dve', lambda e: e.tensor_reduce(out=mx[:, 2, :], in_=G2[:], axis=AX.X, op=ALU.max), reads=['G2'], writes=['mx2'])
                    A('dve', lambda e: e.tensor_tensor(out=Eq[:], in0=Gs[:], in1=mx[:, 2, :].unsqueeze(2).to_broadcast(bshape), op=ALU.is_ge), reads=['Gs', 'mx2'], writes=['Eq'])
                    A('dve', lambda e, g=g: e.tensor_tensor(out=Eq[:], in0=Eq[:], in1=pastf[:, g, :].unsqueeze(1).to_broadcast(bshape), op=ALU.mult), reads=['Eq', 'pastf'], writes=['Eq'])
                    A('dve', lambda e, g=g: e.tensor_tensor(out=Eq[:], in0=Eq[:], in1=ownf[:, g, :].unsqueeze(1).to_broadcast(bshape), op=ALU.add), reads=['Eq', 'ownf'], writes=['Eq'])
                    A('dve', lambda e: e.tensor_scalar(out=Msb[:], in0=Eq[:], scalar1=-1.0, scalar2=-NEGM, op0=ALU.add, op1=ALU.mult), reads=['Eq'], writes=['Msb'])
                    pmtv = [pM_b[0:16, 0:512].rearrange("p (a b) -> p a b", a=4), pM2_b[64:80, 0:512].rearrange("p (a b) -> p a b", a=4)]
                    for gi in range(8):
                        par = gi // 4
                        A('pe', lambda e, gi=gi, pv=pmtv[par]: e.transpose(out=pv[:, gi % 4, :], in_=Msb[:, gi, :], identity=ident_b[:]), reads=['Msb', 'ident_b'], writes=[('ps', 1 + par)])
                    A('act', lambda e, j=j, pv=pmtv[0]: e.activation(out=MT[0:16, :, j * 128:(j + 1) * 128], in_=pv, func=AF.Copy), reads=[('ps', 1)], writes=['MT'])
                    A('act', lambda e, j=j, pv=pmtv[1]: e.activation(out=MT[64:80, :, j * 128:(j + 1) * 128], in_=pv, func=AF.Copy), reads=[('ps', 2)], writes=['MT'])
                A('dve', lambda e, g=g: e.tensor_scalar(out=kmeanT[:, :, g:g + 1], in0=ksum[:, :, g:g + 1], scalar1=1.0 / 256, scalar2=None, op0=ALU.mult), reads=['ksum', 'QA'], writes=['kmeanT'])

                SC.mark('g%d_%d_c_gate' % (b, g))
                acnt = [0]
                for h in range(8):
                    hb, hp = (h % 2) * 64, h // 2
                    pob = 5 + (h % 2)
                    po = pbank[pob][0:65, 0:GT]
                    nkt = 2 * g + 2
                    for kt in range(nkt):
                        q0 = 128 if kt == 2 * g + 1 else 0
                        sbank = 3 + (acnt[0] % 2)
                        pk = acnt[0] % 3
                        acnt[0] += 1
                        pss = pbank[sbank][:, q0:GT]
                        A('pe', lambda e, hb=hb, hp=hp, kt=kt, q0=q0, pss=pss: e.matmul(pss, lhsT=KT[hb:hb + 64, hp, kt * 128:(kt + 1) * 128], rhs=QA[hb:hb + 64, hp, q0:GT], start=True, stop=False),
                          reads=[('KT', kt // 2), 'QA'], writes=[('ps', sbank)])
                        A('pe', lambda e, h=h, kt=kt, q0=q0, pss=pss: e.matmul(pss, lhsT=eall[(h % 2) * 64:(h % 2) * 64 + 16, kt // 2, :], rhs=MT[(h % 2) * 64:(h % 2) * 64 + 16, h // 2, q0:GT], start=False, stop=True),
                          reads=['eall', 'MT'], writes=[('ps', sbank)])
                        src = pss
                        srcres = ('ps', sbank)
                        if kt >= 2 * g - 1:
                            sk = acnt[0] % 2
                            if kt == 2 * g - 1:
                                A('dve', lambda e, sk=sk, sbank=sbank, h=h: e.tensor_tensor(out=sbias[sk][:, 0:128], in0=pbank[sbank][:, 0:128], in1=B01[:, h, 128:256], op=ALU.add), reads=[('ps', sbank), 'B01'], writes=[('sbias', sk)])
                                A('dve', lambda e, sk=sk, sbank=sbank: e.tensor_copy(out=sbias[sk][:, 128:256], in_=pbank[sbank][:, 128:256]), reads=[('ps', sbank)], writes=[('sbias', sk)])
                            elif kt == 2 * g:
                                A('dve', lambda e, sk=sk, sbank=sbank, h=h: e.tensor_tensor(out=sbias[sk][:, 0:256], in0=pbank[sbank][:, 0:256], in1=B01[:, h, 0:256], op=ALU.add), reads=[('ps', sbank), 'B01'], writes=[('sbias', sk)])
                            else:
                                A('dve', lambda e, sk=sk, sbank=sbank, h=h: e.tensor_tensor(out=sbias[sk][:, 128:256], in0=pbank[sbank][:, 128:256], in1=B01[:, h, 0:128], op=ALU.add), reads=[('ps', sbank), 'B01'], writes=[('sbias', sk)])
                            src = sbias[sk][:, q0:GT]
                            srcres = ('sbias', sk)
                        A('act', lambda e, pk=pk, q0=q0, src=src: e.activation(out=PT[pk][:, q0:GT], in_=src, func=AF.Exp), reads=[srcres], writes=[('PT', pk)])
                        A('pe', lambda e, pk=pk, q0=q0, kt=kt, h=h, pob=pob, nkt=nkt: e.matmul(pbank[pob][0:65, q0:GT], lhsT=Vt[:, kt, h, :], rhs=PT[pk][:, q0:GT], start=(kt == 0), stop=(kt == nkt - 1)),
                          reads=[('PT', pk), ('Vt', kt // 2), 'Vones'], writes=[('ps', pob)])
                    A('dve', lambda e, pob=pob: e.reciprocal(out=rden[64:65, :], in_=pbank[pob][64:65, 0:GT]), reads=[('ps', pob)], writes=['rden'])
                    A('pe', lambda e: e.matmul(pbank[7][0:64, 0:GT], lhsT=ones_f[64:65, 0:64], rhs=rden[64:65, :], start=True, stop=True), reads=['rden', 'ones_f'], writes=[('ps', 7)])
                    A('act', lambda e: e.activation(out=rbc[:], in_=pbank[7][0:64, 0:GT], func=AF.Copy), reads=[('ps', 7)], writes=['rbc'])
                    A('dve', lambda e, h=h, pob=pob: e.tensor_tensor(out=OT[:, h, :], in0=pbank[pob][0:64, 0:GT], in1=rbc[:], op=ALU.mult), reads=[('ps', pob), 'rbc'], writes=['OT'])

                SC.mark('g%d_%d_d_att' % (b, g))
                for c in range(2):
                    cs = c * 128
                    cin = car[ch_idx % 2]
                    cout = car[(ch_idx + 1) % 2]
                    rin = ('car', ch_idx % 2)
                    rout = ('car', (ch_idx + 1) % 2)
                    ch_idx += 1
                    for hf in range(2):
                        for ri in range(2):
                            A('pe', lambda e, hf=hf, ri=ri, cs=cs: e.matmul(pbank[1 + ri][:, :], lhsT=uTb[:, hf, cs:cs + 128], rhs=Bbig[:, hf, ri, :], start=True, stop=True),
                              reads=['uTb', 'Bbig'], writes=[('ps', 1 + ri)])
                        wr = WinvR[:, hf * 512:(hf + 1) * 512]
                        wi = WinvI[:, hf * 512:(hf + 1) * 512]
                        A('dve', lambda e, hf=hf, wr=wr: e.tensor_tensor(out=Uq[0][:, hf, :], in0=pbank[1][:, :], in1=wr, op=ALU.mult), reads=[('ps', 1), 'WinvR_T'], writes=[('Uq', 0, hf)])
                        A('dve', lambda e, hf=hf, wi=wi: e.scalar_tensor_tensor(out=Uq[1][:, hf, :], in0=pbank[2][:, :], scalar=-1.0, in1=wi, op0=ALU.mult, op1=ALU.mult), reads=[('ps', 2), 'WinvI_T'], writes=[('Uq', 1, hf)])
                        A('dve', lambda e, hf=hf, wr=wr: e.tensor_tensor(out=Uq[2][:, hf, :], in0=pbank[2][:, :], in1=wr, op=ALU.mult), reads=[('ps', 2), 'WinvR_T'], writes=[('Uq', 2, hf)])
                        A('dve', lambda e, hf=hf, wi=wi: e.tensor_tensor(out=Uq[3][:, hf, :], in0=pbank[1][:, :], in1=wi, op=ALU.mult), reads=[('ps', 1), 'WinvI_T'], writes=[('Uq', 3, hf)])
                        for ri in range(2):
                            zb = 3 + ri
                            for gl in range(4):
                                for tt in range(2):
                                    ui = ri * 2 + tt
                                    A('pe', lambda e, zb=zb, gl=gl, ui=ui, hf=hf, tt=tt: e.matmul(pbank[zb][:, gl * 128:(gl + 1) * 128], lhsT=Uq[ui][:, hf, gl * 128:(gl + 1) * 128], rhs=tri_b[:], start=(tt == 0), stop=(tt == 1)),
                                      reads=[('Uq', ui, hf), 'tri_b'], writes=[('ps', zb)])
                        zr = pbank[3][:, :].rearrange("p (a b) -> p a b", a=4)
                        zi = pbank[4][:, :].rearrange("p (a b) -> p a b", a=4)
                        gsl = slice(hf * 4, hf * 4 + 4)
                        for gl in range(4):
                            gp = hf * 4 + gl
                            A('dve', lambda e, gl=gl, gp=gp, zr=zr, cin=cin: e.scalar_tensor_tensor(out=TP[:, 0, gp, :], in0=zr[:, gl, :], scalar=cin[:, 0, gp:gp + 1], in1=WpowR[:, gp, :], op0=ALU.add, op1=ALU.mult), reads=[('ps', 3), rin, 'ptab'], writes=[('TP', hf)])
                            A('dve', lambda e, gl=gl, gp=gp, zi=zi, cin=cin: e.scalar_tensor_tensor(out=TP[:, 1, gp, :], in0=zi[:, gl, :], scalar=cin[:, 1, gp:gp + 1], in1=WpowI[:, gp, :], op0=ALU.add, op1=ALU.mult), reads=[('ps', 4), rin, 'ptab'], writes=[('TP', hf)])
                            A('dve', lambda e, gl=gl, gp=gp, zi=zi, cin=cin: e.scalar_tensor_tensor(out=TP[:, 2, gp, :], in0=zi[:, gl, :], scalar=cin[:, 1, gp:gp + 1], in1=WpowR[:, gp, :], op0=ALU.add, op1=ALU.mult), reads=[('ps', 4), rin, 'ptab'], writes=[('TP', hf)])
                            A('dve', lambda e, gl=gl, gp=gp, zr=zr, cin=cin: e.scalar_tensor_tensor(out=TP[:, 3, gp, :], in0=zr[:, gl, :], scalar=cin[:, 0, gp:gp + 1], in1=WpowI[:, gp, :], op0=ALU.add, op1=ALU.mult), reads=[('ps', 3), rin, 'ptab'], writes=[('TP', hf)])
                        A('dve', lambda e, zr=zr, cin=cin, gsl=gsl: e.tensor_tensor(out=ctmp[:, 0, :], in0=zr[:, :, 127], in1=cin[:, 0, gsl], op=ALU.add), reads=[('ps', 3), rin], writes=['ct0'])
                        A('dve', lambda e, zi=zi, cin=cin, gsl=gsl: e.tensor_tensor(out=ctmp[:, 1, :], in0=zi[:, :, 127], in1=cin[:, 1, gsl], op=ALU.add), reads=[('ps', 4), rin], writes=['ct1'])
                        A('dve', lambda e, gsl=gsl: e.tensor_tensor(out=ctmp[:, 2, :], in0=ctmp[:, 0, :], in1=W128[:, 0, gsl], op=ALU.mult), reads=['ct0', 'W128'], writes=['ct2'])
                        A('dve', lambda e, gsl=gsl: e.tensor_tensor(out=ctmp[:, 3, :], in0=ctmp[:, 1, :], in1=W128[:, 1, gsl], op=ALU.mult), reads=['ct1', 'W128'], writes=['ct3'])
                        A('dve', lambda e, gsl=gsl, cout=cout: e.tensor_tensor(out=cout[:, 0, gsl], in0=ctmp[:, 2, :], in1=ctmp[:, 3, :], op=ALU.subtract), reads=['ct2', 'ct3'], writes=[rout])
                        A('dve', lambda e, gsl=gsl: e.tensor_tensor(out=ctmp[:, 4, :], in0=ctmp[:, 1, :], in1=W128[:, 0, gsl], op=ALU.mult), reads=['ct1', 'W128'], writes=['ct4'])
                        A('dve', lambda e, gsl=gsl: e.tensor_tensor(out=ctmp[:, 5, :], in0=ctmp[:, 0, :], in1=W128[:, 1, gsl], op=ALU.mult), reads=['ct0', 'W128'], writes=['ct5'])
                        A('dve', lambda e, gsl=gsl, cout=cout: e.tensor_tensor(out=cout[:, 1, gsl], in0=ctmp[:, 4, :], in1=ctmp[:, 5, :], op=ALU.add), reads=['ct4', 'ct5'], writes=[rout])
                        terms = [(0, 0), (1, 1), (2, 2), (3, 2)]
                        n_mm = 0
                        for gl in range(4):
                            gp = hf * 4 + gl
                            for (ti, ci_) in terms:
                                A('pe', lambda e, gp=gp, ti=ti, ci_=ci_, n_mm=n_mm: e.matmul(pbank[7][:, 0:128], lhsT=Cterm[:, ci_, gp, :], rhs=TP[:, ti, gp, :], start=(n_mm == 0), stop=(n_mm == 15)),
                                  reads=['Cterm', ('TP', hf)], writes=[('ps', 7)])
                                n_mm += 1
                        A('dve', lambda e, hf=hf, cs=cs: e.scalar_tensor_tensor(out=ysb[:], in0=uT[:, hf, cs:cs + 128], scalar=dT[:, hf:hf + 1], in1=pbank[7][:, 0:128], op0=ALU.mult, op1=ALU.add), reads=['uT', 'dT', ('ps', 7)], writes=['ysb'])
                        A('dve', lambda e: e.tensor_tensor(out=ysq[:], in0=ysb[:], in1=ysb[:], op=ALU.mult), reads=['ysb'], writes=['ysq'])
                        A('dve', lambda e: e.tensor_scalar(out=ysq[:], in0=ysq[:], scalar1=0.044715, scalar2=1.0, op0=ALU.mult, op1=ALU.add), reads=['ysq'], writes=['ysq'])
                        A('dve', lambda e: e.tensor_tensor(out=ysq[:], in0=ysq[:], in1=ysb[:], op=ALU.mult), reads=['ysq', 'ysb'], writes=['ysq'])
                        A('act', lambda e: e.activation(out=ysq[:], in_=ysq[:], func=AF.Sigmoid, scale=1.5957691216057308), reads=['ysq'], writes=['ysq'])
                        A('dve', lambda e, hf=hf, cs=cs: e.tensor_tensor(out=zT[:, hf, cs:cs + 128], in0=ysq[:], in1=ysb[:], op=ALU.mult), reads=['ysq', 'ysb'], writes=['zT'])

                SC.mark('g%d_%d_e_ssm' % (b, g))
                for fc in range(8):
                    kA = wload(din["w_ao"][fc].rearrange("p a b -> p (a b)"), [])
                    wa_v = WS[kA][0:64, :].rearrange("p (a b) -> p a b", a=8)
                    for h in range(8):
                        A('pe', lambda e, h=h, wa_v=wa_v: e.matmul(pbank[1][:, 0:GT], lhsT=wa_v[:, h, :], rhs=OT[:, h, :], start=(h == 0), stop=(h == 7)), reads=[('WS', kA), 'OT'], writes=[('ps', 1)])
                    kG = wload(din["w_glu"][fc].rearrange("p a b c -> p (a b c)"), [])
                    wg_v = WS[kG][:, 0:512].rearrange("p (v a b) -> p v a b", v=2, a=2)
                    for vi in range(2):
                        for kc in range(2):
                            A('pe', lambda e, vi=vi, kc=kc, wg_v=wg_v: e.matmul(pbank[2 + 3 * vi][:, 0:GT], lhsT=wg_v[:, vi, kc, :], rhs=zT[:, kc, :], start=(kc == 0), stop=(kc == 1)), reads=[('WS', kG), 'zT'], writes=[('ps', 2 + 3 * vi)])
                    for gi in range(2):
                        oc = 14 + gi * 8 + fc
                        k = wload(din["w_in"][oc].rearrange("p a b -> p (a b)"), [])
                        wv = WS[k][:, :].rearrange("p (a b) -> p a b", a=8)
                        for kc in range(8):
                            A('pe', lambda e, kc=kc, wv=wv, gi=gi: e.matmul(pbank[3 + gi][:, 0:GT], lhsT=wv[:, kc, :], rhs=hT[:, kc, :], start=(kc == 0), stop=(kc == 7)), reads=[('WS', k), 'hT'], writes=[('ps', 3 + gi)])
                    A('act', lambda e: e.activation(out=sgA[0][:], in_=pbank[3][:, 0:GT], func=AF.Sigmoid), reads=[('ps', 3)], writes=[('sgA', 0)])
                    A('act', lambda e: e.activation(out=sgA[1][:], in_=pbank[4][:, 0:GT], func=AF.Sigmoid), reads=[('ps', 4)], writes=[('sgA', 1)])
                    A('act', lambda e: e.activation(out=sgA[2][:], in_=pbank[5][:, 0:GT], func=AF.Sigmoid), reads=[('ps', 5)], writes=[('sgA', 2)])
                    A('dve', lambda e: e.tensor_tensor(out=mt1[:], in0=pbank[1][:, 0:GT], in1=sgA[0][:], op=ALU.mult), reads=[('ps', 1), ('sgA', 0)], writes=['mt1'])
                    A('dve', lambda e: e.tensor_tensor(out=mt2[:], in0=pbank[2][:, 0:GT], in1=sgA[2][:], op=ALU.mult), reads=[('ps', 2), ('sgA', 2)], writes=['mt2'])
                    A('pool', lambda e: e.tensor_tensor(out=mt2[:], in0=mt2[:], in1=sgA[1][:], op=ALU.mult), reads=['mt2', ('sgA', 1)], writes=['mt2'])
                    A('pool', lambda e, fc=fc: e.tensor_tensor(out=mergedT[:, fc, :], in0=mt1[:], in1=mt2[:], op=ALU.add), reads=['mt1', 'mt2'], writes=['mergedT'])

                SC.mark('g%d_%d_f_merge' % (b, g))
                for kc in range(8):
                    k = wload(din["w_mix"][kc], [])
                    for j in range(2):
                        for hh in range(2):
                            bk = 1 + j * 2 + hh
                            A('pe', lambda e, kc=kc, k=k, j=j, hh=hh, bk=bk: e.matmul(pbank[bk][:, :], lhsT=mergedT[:, kc, j * 128:(j + 1) * 128], rhs=WS[k][:, hh * 512:(hh + 1) * 512], start=(kc == 0), stop=(kc == 7)),
                              reads=[('WS', k), 'mergedT'], writes=[('ps', bk)])
                for j in range(2):
                    xk = xslots[j]
                    col = 2 + j
                    A('dve', lambda e, col=col: e.memset(stat[:, col:col + 1], 0.0), writes=[('stat', col)])
                    A('dve', lambda e, col=col: e.memset(stat[:, col + 2:col + 3], 0.0), writes=[('stat', col + 2)])
                    A('act', lambda e, j=j, col=col: e.activation(out=rtmp[:, 0:512], in_=pbank[1 + j * 2][:, :], func=AF.Square, accum_out=stat[:, col:col + 1]), reads=[('ps', 1 + j * 2), ('stat', col)], writes=['rtmp', ('stat', col)])
                    A('act', lambda e, j=j, col=col: e.activation(out=rtmp[:, 512:1024], in_=pbank[2 + j * 2][:, :], func=AF.Square, accum_out=stat[:, col + 2:col + 3]), reads=[('ps', 2 + j * 2), ('stat', col + 2)], writes=['rtmp', ('stat', col + 2)])
                    A('dve', lambda e, col=col: e.tensor_tensor(out=stat[:, col:col + 1], in0=stat[:, col:col + 1], in1=stat[:, col + 2:col + 3], op=ALU.add), reads=[('stat', col), ('stat', col + 2)], writes=[('stat', col)])
                    A('dve', lambda e, col=col: e.tensor_scalar(out=stat[:, col:col + 1], in0=stat[:, col:col + 1], scalar1=1.0 / 1024, scalar2=1e-6, op0=ALU.mult, op1=ALU.add), reads=[('stat', col)], writes=[('stat', col)])
                    A('act', lambda e, col=col: e.activation(out=stat[:, col:col + 1], in_=stat[:, col:col + 1], func=AF.Sqrt), reads=[('stat', col)], writes=[('stat', col)])
                    A('dve', lambda e, col=col: e.reciprocal(out=stat[:, col:col + 1], in_=stat[:, col:col + 1]), reads=[('stat', col)], writes=[('stat', col)])
                    for hh in range(2):
                        bk = 1 + j * 2 + hh
                        A('dve', lambda e, hh=hh, bk=bk, col=col: e.scalar_tensor_tensor(out=rtmp[:, hh * 512:(hh + 1) * 512], in0=pbank[bk][:, :], scalar=stat[:, col:col + 1], in1=Grow[:, hh * 512:(hh + 1) * 512], op0=ALU.mult, op1=ALU.mult),
                          reads=[('ps', bk), ('stat', col), 'Grow'], writes=['rtmp'])
                    A('pool', lambda e, xk=xk: e.tensor_tensor(out=XT[xk][:], in0=XT[xk][:], in1=rtmp[:], op=ALU.add), reads=['rtmp', ('XT', xk)], writes=[('XT', xk)])
                    A('pool', lambda e, xk=xk, b=b, j=j, t0=t0: e.dma_start(out=out[b, t0 + j * 128:t0 + (j + 1) * 128, :], in_=XT[xk][:]), reads=[('XT', xk)], writes=[('out', b, 2 * g + j)], dma=True)


        if do_moe:
            SC.barrier(bar_fns)
            big = (S >= 4096)
            TS = min(1024, S)
            NTS = TS // 128
            if big:
                accv = KT[:].rearrange("p a b -> p (a b)").bitcast(F32)[:, 0:NTS * 1024].rearrange("p (a b) -> p a b", a=NTS)
                vflat = Vt[:].rearrange("p a b c -> p (a b c)")
                h2T = vflat[:, 0:8 * TS].rearrange("p (a b) -> p a b", a=8)
                Wg = vflat[:, 8192:12288].rearrange("p (a b) -> p a b", a=8)
                Wu = vflat[:, 12288:16384].rearrange("p (a b) -> p a b", a=8)
            else:
                accv = sb("m_acc", [128, NTS, 1024], F32)[:]
                h2T = sb("m_h2T", [128, 8, TS], BF16)[:]
                Wg = sb("m_Wg", [128, 8, 512], BF16)[:]
                Wu = sb("m_Wu", [128, 8, 512], BF16)[:]
            hidT = TP[:].rearrange("p a b c -> p (a b c)")[:, 0:2048].rearrange("p (a b) -> p a b", a=4)
            Wrt = uTb[:].rearrange("p a b -> p (a b)")[:, 0:288].rearrange("p (a b) -> p a b", a=8)
            Wrt_f = uT[:].rearrange("p a b -> p (a b)")[:, 0:288].rearrange("p (a b) -> p a b", a=8)
            brt = sgA[0][:, 0:36]
            Lg = sgA[0][:, 64:100]
            lem = sgA[1][:, 0:32].rearrange("p (a b) -> p a b", a=4)
            lem2 = sgA[1][:, 32:64]
            e1 = sgA[1][:, 64:96]
            e2 = sgA[1][:, 96:128]
            rs = sgA[2][:, 0:16]
            egt = sgA[2][:, 16:20]
            junk4 = sgA[2][:, 20:24]
            Wtok = sbias[0][:, 0:NTS * 32].rearrange("p (a b) -> p a b", a=NTS)
            ld('sp', Wrt_f[:], din["w_rt"], 'Wrt_f')
            ld('sp', brt[:], bc_rows(din["b_rt"], 128), 'brt')
            A('dve', lambda e: e.tensor_copy(out=Wrt[:], in_=Wrt_f[:]), reads=['Wrt_f'], writes=['Wrt'])
            for b in range(NSEQ):
                gate_rows(5, b, 1)
                for sg in range(S // TS):
                    ts0 = sg * TS
                    A('pool', lambda e: e.memset(accv, 0.0), writes=['acc'])
                    for i in range(NTS):
                        tile = (ts0 // 128) + i
                        xk = xt_cnt[0] % 3
                        xt_cnt[0] += 1
                        ld('sp', XT[xk][:], out[b, tile * 128:(tile + 1) * 128, :], ('XT', xk), reads=[('out', b, tile)])
                        rms_stats('act', XT[xk][:], 0, [('XT', xk)])
                        A('act', lambda e, xk=xk: e.activation(out=xs_b[0][:], in_=XT[xk][:], func=AF.Copy, scale=stat[:, 0:1]), reads=[('XT', xk), ('stat', 0)], writes=[('xs_b', 0)])
                        for fc in range(8):
                            A('pe', lambda e, fc=fc: e.transpose(out=pT_b[:, fc, :], in_=xs_b[0][:, fc * 128:(fc + 1) * 128], identity=ident_b[:]), reads=[('xs_b', 0), 'ident_b'], writes=[('ps', 0)])
                        for fc in range(8):
                            A('act', lambda e, i=i, fc=fc, b=b: e.activation(out=h2T[:, fc, i * 128:(i + 1) * 128], in_=pT_b[:, fc, :], func=AF.Identity, scale=ABt[:, 2, fc, b:b + 1], bias=ABt[:, 3, fc, b:b + 1]),
                              reads=[('ps', 0), 'ABt'], writes=['h2T'])
                        for kc in range(8):
                            A('pe', lambda e, kc=kc, i=i: e.matmul(pbank[3][:, 0:36], lhsT=h2T[:, kc, i * 128:(i + 1) * 128], rhs=Wrt[:, kc, :], start=(kc == 0), stop=(kc == 7)), reads=['h2T', 'Wrt'], writes=[('ps', 3)])
                        A('dve', lambda e: e.tensor_tensor(out=Lg[:], in0=pbank[3][:, 0:36], in1=brt[:], op=ALU.add), reads=[('ps', 3), 'brt'], writes=['Lg'])
                        A('dve', lambda e: e.tensor_reduce(out=rs[:, 0:1], in_=Lg[:, 0:4], axis=AX.X, op=ALU.max), reads=['Lg'], writes=['rs0'])
                        A('dve', lambda e: e.tensor_tensor(out=egt[:], in0=Lg[:, 0:4], in1=rs[:, 0:1].to_broadcast([128, 4]), op=ALU.is_ge), reads=['Lg', 'rs0'], writes=['egt'])
                        A('dve', lambda e: e.tensor_scalar(out=rs[:, 1:2], in0=rs[:, 0:1], scalar1=-1.0, scalar2=None, op0=ALU.mult), reads=['rs0'], writes=['rs1'])
                        A('dve', lambda e: e.memset(rs[:, 2:3], 0.0), writes=['rs2'])
                        A('act', lambda e: e.activation(out=junk4[:], in_=Lg[:, 0:4], func=AF.Exp, bias=rs[:, 1:2], accum_out=rs[:, 2:3]), reads=['Lg', 'rs1', 'rs2'], writes=['rs2', 'junk4'])
                        A('dve', lambda e: e.reciprocal(out=rs[:, 3:4], in_=rs[:, 2:3]), reads=['rs2'], writes=['rs3'])
                        A('dve', lambda e: e.tensor_scalar(out=egt[:], in0=egt[:], scalar1=BIG, scalar2=-BIG, op0=ALU.mult, op1=ALU.add), reads=['egt'], writes=['egt'])
                        A('dve', lambda e: e.tensor_tensor(out=lem[:], in0=Lg[:, 4:36].rearrange("p (a b) -> p a b", a=4), in1=egt[:].unsqueeze(2).to_broadcast([128, 4, 8]), op=ALU.add), reads=['Lg', 'egt'], writes=['lem'])
                        lemf = lem[:].rearrange("p a b -> p (a b)")
                        A('dve', lambda e, lemf=lemf: e.tensor_reduce(out=rs[:, 4:5], in_=lemf, axis=AX.X, op=ALU.max), reads=['lem'], writes=['rs4'])
                        A('dve', lambda e, lemf=lemf: e.tensor_tensor(out=e1[:], in0=lemf, in1=rs[:, 4:5].to_broadcast([128, 32]), op=ALU.is_ge), reads=['lem', 'rs4'], writes=['e1'])
                        A('dve', lambda e, lemf=lemf: e.scalar_tensor_tensor(out=lem2[:], in0=e1[:], scalar=-BIG, in1=lemf, op0=ALU.mult, op1=ALU.add), reads=['e1', 'lem'], writes=['lem2'])
                        A('dve', lambda e: e.tensor_reduce(out=rs[:, 5:6], in_=lem2[:], axis=AX.X, op=ALU.max), reads=['lem2'], writes=['rs5'])
                        A('dve', lambda e: e.tensor_tensor(out=e2[:], in0=lem2[:], in1=rs[:, 5:6].to_broadcast([128, 32]), op=ALU.is_ge), reads=['lem2', 'rs5'], writes=['e2'])
                        A('dve', lambda e: e.tensor_tensor(out=rs[:, 6:7], in0=rs[:, 5:6], in1=rs[:, 4:5], op=ALU.subtract), reads=['rs5', 'rs4'], writes=['rs6'])
                        A('act', lambda e: e.activation(out=rs[:, 7:8], in_=rs[:, 6:7], func=AF.Exp), reads=['rs6'], writes=['rs7'])
                        A('dve', lambda e: e.tensor_scalar(out=rs[:, 8:9], in0=rs[:, 7:8], scalar1=1.0, scalar2=None, op0=ALU.add), reads=['rs7'], writes=['rs8'])
                        A('dve', lambda e: e.reciprocal(out=rs[:, 9:10], in_=rs[:, 8:9]), reads=['rs8'], writes=['rs9'])
                        A('dve', lambda e: e.tensor_tensor(out=rs[:, 10:11], in0=rs[:, 9:10], in1=rs[:, 3:4], op=ALU.mult), reads=['rs9', 'rs3'], writes=['rs10'])
                        A('dve', lambda e: e.tensor_tensor(out=rs[:, 11:12], in0=rs[:, 3:4], in1=rs[:, 10:11], op=ALU.subtract), reads=['rs3', 'rs10'], writes=['rs11'])
                        A('dve', lambda e: e.tensor_scalar(out=e1[:], in0=e1[:], scalar1=rs[:, 10:11], scalar2=None, op0=ALU.mult), reads=['e1', 'rs10'], writes=['e1'])
                        A('dve', lambda e, i=i: e.scalar_tensor_tensor(out=Wtok[:, i, :], in0=e2[:], scalar=rs[:, 11:12], in1=e1[:], op0=ALU.mult, op1=ALU.add), reads=['e2', 'rs11', 'e1'], writes=['Wtok'])
                    for ex in range(32):
                        def wchunk(src, dst, tag):
                            kf = ws_cnt[0] % len(WSF)
                            ws_cnt[0] += 1
                            A('sp', lambda e, kf=kf, src=src: e.dma_start(out=WSF[kf][:, :], in_=src), writes=[('WSF', kf)], dma=True)
                            A('pool', lambda e, kf=kf, dst=dst: e.tensor_copy(out=dst, in_=WSF[kf][:, :]), reads=[('WSF', kf)], writes=[tag])
                        for c4 in range(4):
                            wchunk(din["w_eg"][ex, :, 2 * c4:2 * c4 + 2, :].rearrange("p a b -> p (a b)"), Wg[:, 2 * c4:2 * c4 + 2, :].rearrange("p a b -> p (a b)"), 'Wg')
                            wchunk(din["w_eu"][ex, :, 2 * c4:2 * c4 + 2, :].rearrange("p a b -> p (a b)"), Wu[:, 2 * c4:2 * c4 + 2, :].rearrange("p a b -> p (a b)"), 'Wu')
                        for c4 in range(4):
                            wchunk(din["w_ed"][ex, :, c4, :], WS[c4][:, :], ('WS', c4))
                        for q in range(TS // 512):
                            qs = slice(q * 512, (q + 1) * 512)
                            for mc in range(4):
                                bg, bu = 1 + (mc % 2), 3 + (mc % 2)
                                for kc in range(8):
                                    A('pe', lambda e, kc=kc, mc=mc, bg=bg, qs=qs: e.matmul(pbank[bg][:, :], lhsT=Wg[:, kc, mc * 128:(mc + 1) * 128], rhs=h2T[:, kc, qs], start=(kc == 0), stop=(kc == 7)), reads=['Wg', 'h2T'], writes=[('ps', bg)])
                                for kc in range(8):
                                    A('pe', lambda e, kc=kc, mc=mc, bu=bu, qs=qs: e.matmul(pbank[bu][:, :], lhsT=Wu[:, kc, mc * 128:(mc + 1) * 128], rhs=h2T[:, kc, qs], start=(kc == 0), stop=(kc == 7)), reads=['Wu', 'h2T'], writes=[('ps', bu)])
                                rt = rtmp[:, (mc % 2) * 512:(mc % 2) * 512 + 512]
                                A('act', lambda e, bg=bg, rt=rt: e.activation(out=rt, in_=pbank[bg][:, :], func=AF.Silu), reads=[('ps', bg)], writes=[('rtmpH', mc % 2)])
                                A('dve', lambda e, bu=bu, rt=rt, mc=mc: e.tensor_tensor(out=hidT[:, mc, :], in0=pbank[bu][:, :], in1=rt, op=ALU.mult), reads=[('ps', bu), ('rtmpH', mc % 2)], writes=['hidT'])
                            for t in range(4):
                                tl = q * 4 + t
                                for hh in range(2):
                                    by = 5 + ((t * 2 + hh) % 3)
                                    for mc in range(4):
                                        A('pe', lambda e, mc=mc, t=t, hh=hh, by=by: e.matmul(pbank[by][:, :], lhsT=hidT[:, mc, t * 128:(t + 1) * 128], rhs=WS[mc][:, hh * 512:(hh + 1) * 512], start=(mc == 0), stop=(mc == 3)), reads=['hidT', ('WS', mc)], writes=[('ps', by)])
                                    A('dve', lambda e, tl=tl, hh=hh, by=by, ex=ex: e.scalar_tensor_tensor(out=accv[:, tl, hh * 512:(hh + 1) * 512], in0=pbank[by][:, :], scalar=Wtok[:, tl, ex:ex + 1], in1=accv[:, tl, hh * 512:(hh + 1) * 512], op0=ALU.mult, op1=ALU.add),
                                      reads=[('ps', by), 'Wtok', 'acc'], writes=['acc'])
                    for i in range(NTS):
                        tile = (ts0 // 128) + i
                        xk = xt_cnt[0] % 3
                        xt_cnt[0] += 1
                        ld('sp', XT[xk][:], out[b, tile * 128:(tile + 1) * 128, :], ('XT', xk), reads=[('out', b, tile)])
                        rms_stats('act', accv[:, i, :], 1, ['acc'])
                        A('dve', lambda e, i=i: e.scalar_tensor_tensor(out=rtmp[:], in0=accv[:, i, :], scalar=stat[:, 1:2], in1=Grow[:], op0=ALU.mult, op1=ALU.mult), reads=['acc', ('stat', 1), 'Grow', ('rtmpH', 0), ('rtmpH', 1)], writes=['rtmp', ('rtmpH', 0), ('rtmpH', 1)])
                        A('pool', lambda e, xk=xk: e.tensor_tensor(out=XT[xk][:], in0=XT[xk][:], in1=rtmp[:], op=ALU.add), reads=['rtmp', ('XT', xk)], writes=[('XT', xk)])
                        A('pool', lambda e, xk=xk, b=b, tile=tile: e.dma_start(out=out[b, tile * 128:(tile + 1) * 128, :], in_=XT[xk][:]), reads=[('XT', xk)], writes=[('out', b, tile)], dma=True)

        SC.mark('z_end')
        import os
        if os.environ.get('KSTOP'):
            ks = os.environ['KSTOP']
            SC.ops = SC.ops[:(int(ks) if ks.isdigit() else SC.marks[ks])]
        print('marks', SC.marks, flush=True)
        SC.finalize()
        with nc.Block() as block:
            @block.tensor
            def _(e):
                SC.emit('pe', e)

            @block.scalar
            def _(e):
                SC.emit('act', e)

            @block.vector
            def _(e):
                SC.emit('dve', e)

            @block.gpsimd
            def _(e):
                SC.emit('pool', e)

            @block.sync
            def _(e):
                SC.emit('sp', e)
                SC.final_waits(e)
    return nc


def kernel(**inputs):
    NSEQ, S = 2, 4096
    inp = {k: np.asarray(v) for k, v in inputs.items()}
    w = _layout_weights(inp)
    w.update(_constants())
    nc = build(NSEQ, S)
    in_maps = []
    for c in range(NCORES):
        m = dict(w)
        m["x"] = np.ascontiguousarray(inp["x"][c * NSEQ:(c + 1) * NSEQ])
        m["cT"] = np.ascontiguousarray(inp["c"][c * NSEQ:(c + 1) * NSEQ].reshape(NSEQ, 8, 128).transpose(2, 1, 0))
        in_maps.append(m)
    res = run_bass_kernel_spmd(nc, in_maps, core_ids=list(range(NCORES)))
    return np.concatenate([r["out"] for r in res.results], axis=0).astype(np.float32)
```

```python
import math
from contextlib import ExitStack

import numpy as np
import concourse.bass as bass
import concourse.mybir as mybir
from concourse.bass_utils import run_bass_kernel_spmd

F32 = mybir.dt.float32
BF16 = mybir.dt.bfloat16
ALU = mybir.AluOpType
AF = mybir.ActivationFunctionType
AX = mybir.AxisListType

D = 1024
NCORES = 8
NEGM = -30000.0
BIG = 1.0e30


class Sched:
    def __init__(self, sems, dma_sems):
        self.ops = []
        self.last_w = {}
        self.readers = {}
        self.sems = sems
        self.dma_sems = dma_sems
        self.n_dma = 0
        self.n_dma_sw = 0
        self.waited = {}

    def add(self, eng, fn, reads=(), writes=(), dma=False):
        idx = len(self.ops)
        raw = set()
        other = set()
        for r in reads:
            if r in self.last_w:
                raw.add(self.last_w[r])
        for w in writes:
            if w in self.last_w:
                other.add(self.last_w[w])
            for rd in self.readers.get(w, ()):
                other.add(rd)
        for r in reads:
            self.readers.setdefault(r, []).append(idx)
        for w in writes:
            self.last_w[w] = idx
            self.readers[w] = []
        slot = None
        if dma:
            n_sw = 8
            n_hw = len(self.dma_sems) - n_sw
            if eng == 'pool':
                slot = n_hw + (self.n_dma_sw % n_sw)
                self.n_dma_sw += 1
            else:
                slot = self.n_dma % n_hw
                self.n_dma += 1
        self.ops.append(dict(eng=eng, fn=fn, raw=raw, other=other, dma=dma, slot=slot))
        return idx

    def mark(self, name):
        if not hasattr(self, 'marks'):
            self.marks = {}
        self.marks[name] = len(self.ops)

    def barrier(self, fns):
        first = []
        for e, fn in fns.items():
            first.append(self.add(e, fn, dma=(e == 'sp')))
        last_dma = {}
        for i, o in enumerate(self.ops):
            if o['dma']:
                last_dma[o['slot']] = i
        extra = set(first) | set(last_dma.values())
        for e, fn in fns.items():
            idx = self.add(e, fn, dma=(e == 'sp'))
            self.ops[idx]['raw'] |= extra

    def finalize(self):
        ops = self.ops
        for i, o in enumerate(ops):
            deps = set()
            for d in o['raw']:
                od = ops[d]
                if od['dma'] or od['eng'] != o['eng'] or o['eng'] != 'pe':
                    deps.add(d)
            for d in o['other']:
                od = ops[d]
                if od['dma'] or od['eng'] != o['eng']:
                    deps.add(d)
            deps.discard(i)
            o['deps'] = deps
        prev_slot_op = [None] * len(self.dma_sems)
        for i, o in enumerate(ops):
            if o['dma']:
                s = o['slot']
                if prev_slot_op[s] is not None:
                    o['deps'].add(prev_slot_op[s])
                prev_slot_op[s] = i
        needed = set()
        for o in ops:
            needed |= o['deps']
        cnt = {e: 0 for e in self.sems}
        dcnt = [0] * len(self.dma_sems)
        for i, o in enumerate(ops):
            if o['dma']:
                s = o['slot']
                dcnt[s] += 16
                o['tok'] = (('d', s), dcnt[s])
                o['signal'] = True
            elif i in needed:
                cnt[o['eng']] += 1
                o['tok'] = (('e', o['eng']), cnt[o['eng']])
                o['signal'] = True
            else:
                o['tok'] = None
                o['signal'] = False

    def _sem(self, k):
        return self.dma_sems[k[1]] if k[0] == 'd' else self.sems[k[1]]

    def emit(self, eng_name, eng):
        ops = self.ops
        waited = self.waited.setdefault(eng_name, {})
        for o in ops:
            if o['eng'] != eng_name:
                continue
            need = {}
            for d in o['deps']:
                k, v = ops[d]['tok']
                if waited.get(k, 0) >= v:
                    continue
                if need.get(k, 0) < v:
                    need[k] = v
            for k, v in need.items():
                eng.wait_ge(self._sem(k), v)
                waited[k] = v
            ins = o['fn'](eng)
            if o['signal']:
                ins.then_inc(self._sem(o['tok'][0]), 16 if o['dma'] else 1)

    def final_waits(self, eng):
        cnt = {}
        for o in self.ops:
            if o['tok'] is not None:
                k, v = o['tok']
                if cnt.get(k, 0) < v:
                    cnt[k] = v
        for k, v in cnt.items():
            eng.wait_ge(self._sem(k), v)


def _t5_bucket_np(d):
    n = np.maximum(d, 0)
    nf = np.maximum(n, 1).astype(np.float32)
    large = 16 + (np.log(nf / np.float32(16)) / np.float32(math.log(128 / 16)) * np.float32(16)).astype(np.int32)
    large = np.minimum(large, 31)
    return np.where(n < 16, n, large)


def _constants():
    c = {}
    dd = np.arange(-127, 257)
    bk = _t5_bucket_np(dd)
    oh = np.zeros((33, 384), np.float32)
    for j, d in enumerate(dd):
        if d < 0:
            oh[32, j] = 1.0
        else:
            oh[bk[j], j] = 1.0
    c["c_onehot"] = oh
    c["c_ident"] = np.eye(128, dtype=np.float32)
    s = np.arange(128)
    c["c_tri"] = (s[:, None] <= s[None, :]).astype(np.float32)
    e = np.zeros((16, 16, 128), np.float32)
    for n in range(16):
        e[n, n, :] = 1.0
    c["c_eall"] = e
    qb = np.arange(16)[:, None]
    n = np.arange(16)[None, :]
    c["c_past"] = (n < qb).astype(np.float32).reshape(1, 256)
    c["c_pneg"] = ((n < qb).astype(np.float32) - 1.0).reshape(1, 256) * np.float32(BIG)
    c["c_own"] = (n == qb).astype(np.float32).reshape(1, 256)
    c["c_tidx"] = np.tile(np.arange(128, dtype=np.float32)[None, :], (1, 1))
    return c


def _layout_weights(inp):
    w = {}
    f32 = np.float32
    w_ada = inp["w_ada"][0]
    w["w_ada"] = np.ascontiguousarray(w_ada.reshape(8, 128, 48, 128).transpose(2, 1, 0, 3))
    b_ada = inp["b_ada"][0]
    w["b_adaT"] = np.ascontiguousarray(b_ada.reshape(48, 128).T)
    w["b_ada_row"] = np.ascontiguousarray(b_ada.reshape(1, 6144))
    gT = np.stack([inp["g_pre_mix"][0].reshape(8, 128).T, inp["g_pre_ffn"][0].reshape(8, 128).T], axis=1)
    w["gpreT"] = np.ascontiguousarray(gT)
    w["gpost_rows"] = np.ascontiguousarray(np.stack([inp["g_post_mix"][0], inp["g_post_ffn"][0]])[None])
    w_in = inp["w_in"][0]
    w["w_in"] = np.ascontiguousarray(w_in.reshape(8, 128, 30, 128).transpose(2, 1, 0, 3))
    wao = inp["w_att_out"][0]
    w["w_ao"] = np.ascontiguousarray(wao.reshape(8, 64, 8, 128).transpose(2, 1, 0, 3))
    wgv = inp["w_glu_val"][0].reshape(2, 128, 8, 128).transpose(2, 1, 0, 3)
    wgg = inp["w_glu_gate"][0].reshape(2, 128, 8, 128).transpose(2, 1, 0, 3)
    w["w_glu"] = np.ascontiguousarray(np.stack([wgv, wgg], axis=2))
    w["w_mix"] = np.ascontiguousarray(inp["w_mix_out"][0].reshape(8, 128, 1024))
    lr = inp["ssm_lambda_re"][0]
    li = inp["ssm_lambda_im"][0]
    ld = inp["ssm_log_dt"][0]
    ldf = np.repeat(ld[:, None], 64, axis=1)

    def fm(a):
        return a.reshape(8, 2, 64).transpose(1, 2, 0).reshape(128, 8)
    w["ssmP"] = np.ascontiguousarray(np.stack([fm(lr), fm(li), fm(ldf)], axis=1))
    w["ssmF"] = np.ascontiguousarray(np.stack([lr.reshape(-1), li.reshape(-1), ldf.reshape(-1)])[None])
    br = inp["ssm_b_re"][0]
    bi = inp["ssm_b_im"][0]
    bbig = np.zeros((128, 2, 2, 512), f32)
    for g in range(16):
        hf, gl = g // 8, g % 8
        bbig[gl * 16:(gl + 1) * 16, hf, 0, gl * 64:(gl + 1) * 64] = br[g].T
        bbig[gl * 16:(gl + 1) * 16, hf, 1, gl * 64:(gl + 1) * 64] = bi[g].T
    w["bbig"] = bbig
    cr = inp["ssm_c_re"][0]
    ci = inp["ssm_c_im"][0]
    cpad = np.zeros((128, 2, 8, 128), f32)
    for g in range(16):
        gp, g2 = g // 2, g % 2
        gl = g % 8
        cpad[g2 * 64:(g2 + 1) * 64, 0, gp, gl * 16:(gl + 1) * 16] = cr[g].T
        cpad[g2 * 64:(g2 + 1) * 64, 1, gp, gl * 16:(gl + 1) * 16] = ci[g].T
    w["cpad"] = cpad
    w["ssm_dT"] = np.ascontiguousarray(inp["ssm_d"][0].reshape(2, 128).T)
    w["rel_bias"] = np.ascontiguousarray(inp["rel_bias"])
    w["rb31"] = np.ascontiguousarray(inp["rel_bias"][31:32, :])
    wr = np.concatenate([inp["w_router_group"][0], inp["w_router_expert"][0]], axis=1)
    w["w_rt"] = np.ascontiguousarray(wr.reshape(8, 128, 36).transpose(1, 0, 2))
    w["b_rt"] = np.ascontiguousarray(np.concatenate([inp["b_router_group"][0], inp["b_router_expert"][0]])[None])
    w["w_eg"] = np.ascontiguousarray(inp["w_exp_gate"][0].reshape(32, 8, 128, 512).transpose(0, 2, 1, 3))
    w["w_eu"] = np.ascontiguousarray(inp["w_exp_up"][0].reshape(32, 8, 128, 512).transpose(0, 2, 1, 3))
    w["w_ed"] = np.ascontiguousarray(inp["w_exp_down"][0].reshape(32, 4, 128, 1024).transpose(0, 2, 1, 3))
    return w


IN_SHAPES = {
    "w_ada": [48, 128, 8, 128], "b_adaT": [128, 48], "b_ada_row": [1, 6144], "gpreT": [128, 2, 8],
    "gpost_rows": [1, 2, 1024], "w_in": [30, 128, 8, 128], "w_ao": [8, 64, 8, 128], "w_glu": [8, 128, 2, 2, 128],
    "w_mix": [8, 128, 1024], "ssmP": [128, 3, 8], "ssmF": [1, 3, 1024], "bbig": [128, 2, 2, 512],
    "cpad": [128, 2, 8, 128], "ssm_dT": [128, 2], "rel_bias": [32, 8], "rb31": [1, 8],
    "w_rt": [128, 8, 36], "b_rt": [1, 36], "w_eg": [32, 128, 8, 512], "w_eu": [32, 128, 8, 512],
    "w_ed": [32, 128, 4, 1024],
    "c_onehot": [33, 384], "c_ident": [128, 128], "c_tri": [128, 128], "c_eall": [16, 16, 128],
    "c_past": [1, 256], "c_pneg": [1, 256], "c_own": [1, 256], "c_tidx": [1, 128],
}


def bc_rows(ap_row, nparts):
    pat = [list(x) for x in ap_row.ap]
    pat[0] = [0, nparts]
    return bass.AP(ap_row.tensor, ap_row.offset, pat)


def build(NSEQ, S, do_moe=True, dbg=False):
    GT = 256
    NG = S // GT
    NT = S // 128
    nc = bass.Bass("TRN2", target_bir_lowering=False)
    din = {}
    din["x"] = nc.dram_tensor("x", [NSEQ, S, D], F32, kind="ExternalInput").ap()
    din["cT"] = nc.dram_tensor("cT", [128, 8, NSEQ], F32, kind="ExternalInput").ap()
    for k, shp in IN_SHAPES.items():
        din[k] = nc.dram_tensor(k, shp, F32, kind="ExternalInput").ap()
    out = nc.dram_tensor("out", [NSEQ, S, D], F32, kind="ExternalOutput").ap()
    dbg_out = {}
    s_win = nc.dram_tensor("s_win", [30, 128, 1024], BF16).ap()
    s_wao = nc.dram_tensor("s_wao", [8, 64, 1024], BF16).ap()
    s_wglu = nc.dram_tensor("s_wglu", [8, 128, 512], BF16).ap()
    s_wmix = nc.dram_tensor("s_wmix", [8, 128, 1024], BF16).ap()
    s_weg = nc.dram_tensor("s_weg", [32, 128, 4096], BF16).ap()
    s_weu = nc.dram_tensor("s_weu", [32, 128, 4096], BF16).ap()
    s_wed = nc.dram_tensor("s_wed", [32, 128, 4096], BF16).ap()
    s_bias = nc.dram_tensor("s_bias", [8, 128, 384], F32).ap()

    es = ExitStack()
    with es:
        def sb(name, shape, dt):
            return es.enter_context(nc.sbuf_tensor("sb_" + name, shape, dt))

        def ps(name, shape, dt):
            return es.enter_context(nc.psum_tensor(name, shape, dt))

        sems = {e: es.enter_context(nc.semaphore("s_" + e)) for e in ['pe', 'act', 'dve', 'pool', 'sp']}
        dsems = [es.enter_context(nc.semaphore("d%d" % i)) for i in range(24)]
        SC = Sched(sems, dsems)
        A = SC.add

        pbank = [ps("pb%d" % i, [128, 512], F32) for i in range(8)]

        def pbf(i):
            return pbank[i]


        ident_b = sb("ident_b", [128, 128], BF16)
        ident_f = sb("ident_f", [128, 128], F32)
        tri_b = sb("tri_b", [128, 128], BF16)
        eall = sb("eall", [80, 16, 128], BF16)
        pastf = sb("pastf", [128, 16, 16], F32)
        pneg = sb("pneg", [128, 16, 16], F32)
        ownf = sb("ownf", [128, 16, 16], F32)
        cact = sb("cact", [128, 8, NSEQ], F32)
        badaT = sb("badaT", [128, 48], F32)
        gpreT = sb("gpreT", [128, 2, 8], F32)
        modT = sb("modT", [128, 4, 8, NSEQ], F32)
        ABt = sb("ABt", [128, 4, 8, NSEQ], F32)
        Grow = sb("Grow", [128, 1024], F32)
        ones_f = sb("ones_f", [128, 64], F32)
        ssmP = sb("ssmP", [128, 3, 8], F32)
        WpowR = sb("WpowR", [128, 8, 128], F32)
        WpowI = sb("WpowI", [128, 8, 128], F32)
        WinvR = sb("WinvR", [128, 1024], F32)
        WinvI = sb("WinvI", [128, 1024], F32)
        W128 = sb("W128", [128, 2, 8], F32)
        Bbig = sb("Bbig", [128, 2, 2, 512], BF16)
        Cterm = sb("Cterm", [128, 3, 8, 128], BF16)
        dT = sb("dT", [128, 2], F32)
        halfpi = sb("halfpi", [128, 1], F32)
        B01 = sb("B01", [128, 8, 256], F32)
        rb_a = sb("rb_a", [32, 8], F32)
        rb_b = sb("rb_b", [32, 8], F32)
        dmy = sb("dmy", [128, 8], F32)
        KT = sb("KT", [128, 4, S], BF16)
        Vt = sb("Vt", [128, NT, 8, 65], BF16)
        ksum = sb("ksum", [128, 4, 16], F32)
        kmeanT = sb("kmeanT", [128, 4, 16], BF16)
        XT = [sb("XT%d" % i, [128, 1024], F32) for i in range(3)]
        xs_b = [sb("xs_b%d" % i, [128, 1024], BF16) for i in range(2)]
        hT = sb("hT", [128, 8, GT], BF16)
        WS = [sb("WS%d" % i, [128, 1024], BF16) for i in range(6)]
        WSF = [sb("WSF%d" % i, [128, 1024], F32) for i in range(3)]
        QA = sb("QA", [128, 4, GT], BF16)
        MT = sb("MT", [80, 4, GT], BF16)
        Gs = sb("Gs", [128, 8, 16], F32)
        G2 = sb("G2", [128, 8, 16], F32)
        Eq = sb("Eq", [128, 8, 16], F32)
        mx = sb("mx", [128, 3, 8], F32)
        Msb = sb("Msb", [128, 8, 16], BF16)
        sbias = [sb("sbias%d" % i, [128, GT], F32) for i in range(2)]
        PT = [sb("PT%d" % i, [128, GT], BF16) for i in range(3)]
        OT = sb("OT", [64, 8, GT], BF16)
        rden = sb("rden", [65, GT], F32)
        rbc = sb("rbc", [64, GT], F32)
        uT = sb("uT", [128, 2, GT], F32)
        uTb = sb("uTb", [128, 2, GT], BF16)
        Uq = [sb("Uq%d" % i, [128, 2, 512], BF16) for i in range(4)]
        TP = sb("TP", [128, 4, 8, 128], BF16)
        car = [sb("car%d" % i, [128, 2, 8], F32) for i in range(2)]
        ctmp = sb("ctmp", [128, 6, 4], F32)
        ysb = sb("ysb", [128, 128], F32)
        ysq = sb("ysq", [128, 128], F32)
        zT = sb("zT", [128, 2, GT], BF16)
        sgA = [sb("sgA%d" % i, [128, GT], F32) for i in range(3)]
        mt1 = sb("mt1", [128, GT], F32)
        mt2 = sb("mt2", [128, GT], F32)
        mergedT = sb("mergedT", [128, 8, GT], BF16)
        stat = sb("stat", [128, 8], F32)
        rtmp = sb("rtmp", [128, 1024], F32)

        tA = []
        for t_ in (XT[0], XT[1], XT[2], rtmp):
            tA.append(t_[:, 0:512])
            tA.append(t_[:, 512:1024])
        TPf = TP[:].rearrange("p a b c -> p (a b c)").bitcast(F32)
        FRh = TPf[:, 0:1536].rearrange("p (a b) -> p a b", a=3)
        Bf_re = Uq[0][:].rearrange("p a b -> p (a b)").bitcast(F32)
        Bf_im = Uq[1][:].rearrange("p a b -> p (a b)").bitcast(F32)
        Cf_a = Uq[2][:].rearrange("p a b -> p (a b)").bitcast(F32)
        Cf_b = Uq[3][:].rearrange("p a b -> p (a b)").bitcast(F32)
        rbl = mergedT[:].rearrange("p a b -> p (a b)").bitcast(F32)[0:33, :].rearrange("p (a b) -> p a b", a=8)
        oneh = hT[:].rearrange("p a b -> p (a b)").bitcast(F32)[0:33, 0:384]

        def ld(eng, dst, src, wname, reads=()):
            A(eng, lambda e, dst=dst, src=src: e.dma_start(out=dst, in_=src), reads=list(reads), writes=[wname], dma=True)

        ld('sp', ident_f[:], din["c_ident"], 'ident_f')
        ld('pool', ident_b[:], din["c_ident"], 'ident_b')
        ld('pool', tri_b[:], din["c_tri"], 'tri_b')
        ld('pool', eall[0:16], din["c_eall"], 'eall')
        ld('pool', eall[64:80], din["c_eall"], 'eall')
        ld('sp', pastf[:].rearrange("p a b -> p (a b)"), bc_rows(din["c_past"], 128), 'pastf')
        ld('sp', pneg[:].rearrange("p a b -> p (a b)"), bc_rows(din["c_pneg"], 128), 'pneg')
        ld('sp', ownf[:].rearrange("p a b -> p (a b)"), bc_rows(din["c_own"], 128), 'ownf')
        ld('sp', cact[:], din["cT"], 'cact')
        ld('sp', badaT[:], din["b_adaT"], 'badaT')
        ld('sp', gpreT[:], din["gpreT"], 'gpreT')
        ld('sp', ssmP[:], din["ssmP"], 'ssmP')
        ld('sp', dT[:], din["ssm_dT"], 'dT')
        A('dve', lambda e: e.memset(ones_f[:], 1.0), writes=['ones_f'])
        A('dve', lambda e: e.memset(halfpi[:], math.pi / 2), writes=['halfpi'])
        A('dve', lambda e: e.memset(dmy[:], 0.0), writes=['dmy'])
        A('act', lambda e: e.activation(out=cact[:], in_=cact[:], func=AF.Silu), reads=['cact'], writes=['cact'])

        SC.mark('a_casts')
        WAv = [(rtmp[:].rearrange("p (a b) -> p a b", a=8), 'rtmp'), (XT[0][:].rearrange("p (a b) -> p a b", a=8), ('XT', 0))]
        CBv, CBtag = XT[2][:].rearrange("p (a b) -> p a b", a=8), ('XT', 2)
        rowtmp, rowtag = XT[1], ('XT', 1)
        wa_cnt = [0]

        def load_wa(pc):
            k = wa_cnt[0] % 2
            wa_cnt[0] += 1
            ld('sp', WAv[k][0], din["w_ada"][pc], WAv[k][1])
            return k

        fm_js = [0, 1, 3, 4]
        for mi, j in enumerate(fm_js):
            for fc in range(8):
                k = load_wa(j * 8 + fc)
                for kc in range(8):
                    A('pe', lambda e, k=k, kc=kc: e.matmul(pbank[0][:, 0:NSEQ], lhsT=WAv[k][0][:, kc, :], rhs=cact[:, kc, :], start=(kc == 0), stop=(kc == 7)),
                      reads=[WAv[k][1], 'cact'], writes=[('ps', 0)])
                A('act', lambda e, mi=mi, fc=fc, j=j: e.activation(out=modT[:, mi, fc, :], in_=pbank[0][:, 0:NSEQ], func=AF.Identity, bias=badaT[:, j * 8 + fc:j * 8 + fc + 1]),
                  reads=[('ps', 0), 'badaT'], writes=['modT'])
        for (ai, si, hi, gi) in [(0, 1, 0, 0), (2, 3, 2, 1)]:
            A('dve', lambda e, ai=ai, si=si, gi=gi: e.scalar_tensor_tensor(out=ABt[:, ai], in0=modT[:, si], scalar=1.0, in1=gpreT[:, gi, :].unsqueeze(2).to_broadcast([128, 8, NSEQ]), op0=ALU.add, op1=ALU.mult),
              reads=['modT', 'gpreT'], writes=['ABt'])
            A('dve', lambda e, ai=ai, hi=hi: e.tensor_copy(out=ABt[:, ai + 1], in_=modT[:, hi]), reads=['modT'], writes=['ABt'])

        def gate_rows(j, b, gidx):
            A('dve', lambda e, b=b: e.tensor_copy(out=CBv, in_=cact[:, :, b:b + 1].to_broadcast([128, 8, 128])), reads=['cact'], writes=[CBtag])
            ld('sp', rowtmp[:], bc_rows(din["b_ada_row"][:, j * 1024:(j + 1) * 1024], 128), rowtag)
            for pcl in range(8):
                k = load_wa(j * 8 + pcl)
                for kc in range(8):
                    A('pe', lambda e, k=k, kc=kc: e.matmul(pbank[0][:, 0:128], lhsT=CBv[:, kc, :], rhs=WAv[k][0][:, kc, :], start=(kc == 0), stop=(kc == 7)),
                      reads=[WAv[k][1], CBtag], writes=[('ps', 0)])
                A('dve', lambda e, pcl=pcl: e.tensor_tensor(out=Grow[:, pcl * 128:(pcl + 1) * 128], in0=pbank[0][:, 0:128], in1=rowtmp[:, pcl * 128:(pcl + 1) * 128], op=ALU.add),
                  reads=[('ps', 0), rowtag], writes=['Grow'])
            ld('sp', rowtmp[:], bc_rows(din["gpost_rows"][:, gidx, :], 128), rowtag, reads=['Grow'])
            A('dve', lambda e: e.tensor_tensor(out=Grow[:], in0=Grow[:], in1=rowtmp[:], op=ALU.mult), reads=['Grow', rowtag], writes=['Grow'])

        SC.mark('b_adaln')
        def abar_small(lr, li, ldt, n, outs, sign, tg):
            t_dt, t_m, t_s, t_c = [tA[4 + i][:, 0:n] for i in range(4)]
            o_re, o_im = outs
            A('act', lambda e: e.activation(out=t_dt, in_=ldt, func=AF.Exp), reads=[tg, 'abar_o'], writes=['tA4'])
            A('dve', lambda e: e.tensor_tensor(out=t_m, in0=lr, in1=t_dt, op=ALU.mult), reads=['tA4', tg], writes=['tA5'])
            A('act', lambda e: e.activation(out=t_m, in_=t_m, func=AF.Exp, scale=sign / 32.0), reads=['tA5'], writes=['tA5'])
            A('dve', lambda e: e.tensor_tensor(out=t_s, in0=li, in1=t_dt, op=ALU.mult), reads=['tA4', tg], writes=['tA6'])
            A('act', lambda e: e.activation(out=t_c, in_=t_s, func=AF.Sin, scale=sign / 32.0, bias=halfpi[:]), reads=['tA6', 'halfpi'], writes=['tA7'])
            A('act', lambda e: e.activation(out=t_s, in_=t_s, func=AF.Sin, scale=sign / 32.0), reads=['tA6'], writes=['tA6'])
            A('dve', lambda e: e.tensor_tensor(out=o_re, in0=t_m, in1=t_c, op=ALU.mult), reads=['tA5', 'tA7'], writes=['abar_o'])
            A('dve', lambda e: e.tensor_tensor(out=o_im, in0=t_m, in1=t_s, op=ALU.mult), reads=['tA5', 'tA6'], writes=['abar_o'])
            for _ in range(5):
                A('dve', lambda e: e.tensor_tensor(out=t_c, in0=o_re, in1=o_re, op=ALU.mult), reads=['abar_o'], writes=['tA7'])
                A('dve', lambda e: e.tensor_tensor(out=t_s, in0=o_im, in1=o_im, op=ALU.mult), reads=['abar_o'], writes=['tA6'])
                A('dve', lambda e: e.tensor_tensor(out=t_m, in0=o_re, in1=o_im, op=ALU.mult), reads=['abar_o'], writes=['tA5'])
                A('dve', lambda e: e.tensor_tensor(out=o_re, in0=t_c, in1=t_s, op=ALU.subtract), reads=['tA7', 'tA6'], writes=['abar_o'])
                A('dve', lambda e: e.tensor_scalar(out=o_im, in0=t_m, scalar1=2.0, scalar2=None, op0=ALU.mult), reads=['tA5'], writes=['abar_o'])

        def cmul_bc(o_re, o_im, a_re, a_im, b_re, b_im, tg_r, tg_w, tmp0, tmp1):
            A('dve', lambda e: e.tensor_tensor(out=tmp0, in0=a_re, in1=b_re, op=ALU.mult), reads=tg_r, writes=['cm0'])
            A('dve', lambda e: e.tensor_tensor(out=tmp1, in0=a_im, in1=b_im, op=ALU.mult), reads=tg_r, writes=['cm1'])
            A('dve', lambda e: e.tensor_tensor(out=o_re, in0=tmp0, in1=tmp1, op=ALU.subtract), reads=['cm0', 'cm1'], writes=tg_w)
            A('dve', lambda e: e.tensor_tensor(out=tmp0, in0=a_re, in1=b_im, op=ALU.mult), reads=tg_r + tg_w, writes=['cm0'])
            A('dve', lambda e: e.tensor_tensor(out=tmp1, in0=a_im, in1=b_re, op=ALU.mult), reads=tg_r + tg_w, writes=['cm1'])
            A('dve', lambda e: e.tensor_tensor(out=o_im, in0=tmp0, in1=tmp1, op=ALU.add), reads=['cm0', 'cm1'], writes=tg_w)

        def power_table(TR, TI, sign):
            ab_re = tA[0][:, 0:8]
            ab_im = tA[0][:, 8:16]
            abar_small(ssmP[:, 0, :], ssmP[:, 1, :], ssmP[:, 2, :], 8, (ab_re, ab_im), sign, 'ssmP')
            A('dve', lambda e: e.memset(TR[:, :, 0:1], 1.0), writes=['ptab'])
            A('dve', lambda e: e.memset(TI[:, :, 0:1], 0.0), writes=['ptab'])
            pw_re = tA[0][:, 16:24]
            pw_im = tA[0][:, 24:32]
            A('dve', lambda e: e.tensor_copy(out=pw_re, in_=ab_re), reads=['abar_o'], writes=['pw'])
            A('dve', lambda e: e.tensor_copy(out=pw_im, in_=ab_im), reads=['abar_o'], writes=['pw'])
            n = 1
            while n < 128:
                shp = [128, 8, n]
                cmul_bc(TR[:, :, n:2 * n], TI[:, :, n:2 * n], TR[:, :, 0:n], TI[:, :, 0:n],
                        pw_re.unsqueeze(2).to_broadcast(shp), pw_im.unsqueeze(2).to_broadcast(shp),
                        ['ptab', 'pw'], ['ptab'],
                        tA[1][:, 0:8 * n].rearrange("p (a b) -> p a b", a=8), tA[2][:, 0:8 * n].rearrange("p (a b) -> p a b", a=8))
                n *= 2
                if n < 128 or sign > 0:
                    cmul_bc(tA[0][:, 32:40], tA[0][:, 40:48], pw_re, pw_im, pw_re, pw_im, ['pw'], ['pw2'], tA[1][:, 0:8], tA[2][:, 0:8])
                    A('dve', lambda e: e.tensor_copy(out=tA[0][:, 16:32], in_=tA[0][:, 32:48]), reads=['pw2'], writes=['pw'])
            return pw_re, pw_im

        pw_re, pw_im = power_table(WpowR, WpowI, 1.0)
        A('dve', lambda e: e.tensor_copy(out=W128[:, 0, :], in_=pw_re), reads=['pw'], writes=['W128'])
        A('dve', lambda e: e.tensor_copy(out=W128[:, 1, :], in_=pw_im), reads=['pw'], writes=['W128'])
        InvR = WinvR[:].rearrange("p (a b) -> p a b", a=8)
        InvI = WinvI[:].rearrange("p (a b) -> p a b", a=8)
        power_table(InvR, InvI, -1.0)
        for T_ in (WinvR, WinvI):
            for q4 in range(2):
                for gl in range(4):
                    gp = q4 * 4 + gl
                    A('pe', lambda e, T_=T_, gp=gp, gl=gl: e.transpose(out=pbank[1][:, gl * 128:(gl + 1) * 128], in_=T_[:, gp * 128:(gp + 1) * 128], identity=ident_f[:]),
                      reads=['ptab', 'ident_f'], writes=[('ps', 1)])
                A('act', lambda e, q4=q4: e.activation(out=tA[3 + q4], in_=pbank[1][:, :], func=AF.Copy), reads=[('ps', 1), 'abar_o'], writes=['tAtr'])
            A('dve', lambda e, T_=T_: e.tensor_copy(out=T_[:, 0:512], in_=tA[3]), reads=['tAtr', 'ptab'], writes=['ptab'])
            A('dve', lambda e, T_=T_: e.tensor_copy(out=T_[:, 512:1024], in_=tA[4]), reads=['tAtr', 'ptab'], writes=['ptab'])
        for hf in range(2):
            ld('sp', FRh, bc_rows(din["ssmF"][:, :, hf * 512:(hf + 1) * 512], 128), 'FR', reads=['Bbig'])
            ld('sp', Bf_re, din["bbig"][:, hf, 0, :], 'Bf', reads=['Bbig'])
            ld('sp', Bf_im, din["bbig"][:, hf, 1, :], 'Bf', reads=['Bbig'])
            abF_re, abF_im = tA[0], tA[1]
            abar_small(FRh[:, 0, :], FRh[:, 1, :], FRh[:, 2, :], 512, (abF_re, abF_im), 1.0, 'FR')
            den, fre, fim, t7 = tA[4], tA[5], tA[6], tA[7]
            lrF, liF = FRh[:, 0, :], FRh[:, 1, :]
            A('dve', lambda e: e.tensor_tensor(out=den, in0=lrF, in1=lrF, op=ALU.mult), reads=['FR', 'abar_o'], writes=['tA4'])
            A('dve', lambda e: e.tensor_tensor(out=t7, in0=liF, in1=liF, op=ALU.mult), reads=['FR', 'abar_o'], writes=['tA7'])
            A('dve', lambda e: e.tensor_tensor(out=den, in0=den, in1=t7, op=ALU.add), reads=['tA4', 'tA7'], writes=['tA4'])
            A('dve', lambda e: e.reciprocal(out=den, in_=den), reads=['tA4'], writes=['tA4'])
            A('dve', lambda e: e.tensor_scalar(out=abF_re, in0=abF_re, scalar1=-1.0, scalar2=None, op0=ALU.add), reads=['abar_o'], writes=['abar_o'])
            A('dve', lambda e: e.tensor_tensor(out=fre, in0=abF_re, in1=lrF, op=ALU.mult), reads=['abar_o', 'FR'], writes=['tA5'])
            A('dve', lambda e: e.tensor_tensor(out=t7, in0=abF_im, in1=liF, op=ALU.mult), reads=['abar_o', 'FR', 'tA4'], writes=['tA7'])
            A('dve', lambda e: e.tensor_tensor(out=fre, in0=fre, in1=t7, op=ALU.add), reads=['tA5', 'tA7'], writes=['tA5'])
            A('dve', lambda e: e.tensor_tensor(out=fre, in0=fre, in1=den, op=ALU.mult), reads=['tA5', 'tA4'], writes=['tA5'])
            A('dve', lambda e: e.tensor_tensor(out=fim, in0=abF_im, in1=lrF, op=ALU.mult), reads=['abar_o', 'FR'], writes=['tA6'])
            A('dve', lambda e: e.tensor_tensor(out=t7, in0=abF_re, in1=liF, op=ALU.mult), reads=['abar_o', 'FR', 'tA5'], writes=['tA7'])
            A('dve', lambda e: e.tensor_tensor(out=fim, in0=fim, in1=t7, op=ALU.subtract), reads=['tA6', 'tA7'], writes=['tA6'])
            A('dve', lambda e: e.tensor_tensor(out=fim, in0=fim, in1=den, op=ALU.mult), reads=['tA6', 'tA4'], writes=['tA6'])
            bq0, bq1 = tA[2], tA[3]
            A('dve', lambda e: e.tensor_tensor(out=bq0, in0=Bf_re, in1=fre, op=ALU.mult), reads=['Bf', 'tA5', 'tAtr', 'ptab'], writes=['bt0'])
            A('dve', lambda e: e.tensor_tensor(out=bq1, in0=Bf_im, in1=fim, op=ALU.mult), reads=['Bf', 'tA6', 'tAtr', 'ptab'], writes=['bt1'])
            A('dve', lambda e, hf=hf: e.tensor_tensor(out=Bbig[:, hf, 0, :], in0=bq0, in1=bq1, op=ALU.subtract), reads=['bt0', 'bt1'], writes=['Bbig'])
            A('dve', lambda e: e.tensor_tensor(out=bq0, in0=Bf_im, in1=fre, op=ALU.mult), reads=['Bf', 'tA5', 'Bbig'], writes=['bt0'])
            A('dve', lambda e: e.tensor_tensor(out=bq1, in0=Bf_re, in1=fim, op=ALU.mult), reads=['Bf', 'tA6', 'Bbig'], writes=['bt1'])
            A('dve', lambda e, hf=hf: e.tensor_tensor(out=Bbig[:, hf, 1, :], in0=bq0, in1=bq1, op=ALU.add), reads=['bt0', 'bt1'], writes=['Bbig'])
        for ri in range(2):
            for hq in range(2):
                cf = Cf_a if hq == 0 else Cf_b
                src = din["cpad"][:, ri, hq * 4:(hq + 1) * 4, :].rearrange("p a b -> p (a b)")
                ld('sp', cf, src, ('Cf', hq), reads=['Bbig'])
                cf3 = cf.rearrange("p (a b) -> p a b", a=4)
                gs = slice(hq * 4, hq * 4 + 4)
                if ri == 0:
                    A('act', lambda e, cf3=cf3, gs=gs: e.activation(out=Cterm[:, 0, gs, :], in_=cf3, func=AF.Copy), reads=[('Cf', hq)], writes=['Cterm'])
                    A('act', lambda e, cf3=cf3, gs=gs: e.activation(out=Cterm[:, 1, gs, :], in_=cf3, func=AF.Copy, scale=-1.0), reads=[('Cf', hq)], writes=['Cterm'])
                else:
                    A('act', lambda e, cf3=cf3, gs=gs: e.activation(out=Cterm[:, 2, gs, :], in_=cf3, func=AF.Copy, scale=-1.0), reads=[('Cf', hq)], writes=['Cterm'])

        SC.mark('c_ssmtab')
        ld('sp', rb_a[:], din["rel_bias"], 'rb_a')
        ld('sp', rb_b[:], bc_rows(din["rb31"], 32), 'rb_b')
        ld('sp', oneh, din["c_onehot"], 'oneh')
        A('dve', lambda e: e.tensor_tensor(out=rb_a[:], in0=rb_a[:], in1=rb_b[:], op=ALU.subtract), reads=['rb_a', 'rb_b'], writes=['rb_a'])
        A('dve', lambda e: e.memset(rbl[32:33, :, :], NEGM), writes=['rbl'])
        A('dve', lambda e: e.tensor_copy(out=rbl[0:32, :, :], in_=rb_a[:].unsqueeze(2).to_broadcast([32, 8, 128])), reads=['rb_a'], writes=['rbl'])
        btmp = xs_b[0][:].bitcast(F32)
        for h in range(8):
            A('pe', lambda e, h=h: e.matmul(pbank[2][:, 0:384], lhsT=rbl[:, h, :], rhs=oneh, start=True, stop=True), reads=['rbl', 'oneh'], writes=[('ps', 2)])
            A('act', lambda e: e.activation(out=btmp[:, 0:384], in_=pbank[2][:, 0:384], func=AF.Copy), reads=[('ps', 2)], writes=['btmp'])
            A('pool', lambda e, h=h: e.dma_start(out=s_bias[h], in_=btmp[:, 0:384]), reads=['btmp'], writes=[('s_bias', h)], dma=True)
            toe = bass.AP(s_bias.tensor, h * 128 * 384 + 127, [[383, 128], [1, 256]])
            A('sp', lambda e, h=h, toe=toe: e.dma_start(out=B01[:, h, :], in_=toe), reads=[('s_bias', h)], writes=['B01'], dma=True)

        A('pool', lambda e: e.memset(kmeanT[:], 0.0), writes=['kmeanT'])
        A('pool', lambda e: e.memset(Vt[:, :, :, 64:65], 1.0), writes=['Vones'])

        SC.mark('d_bias')
        bar_fns = {
            'pe': lambda e: e.matmul(pbank[7][0:1, 0:1], lhsT=ident_f[0:1, 0:1], rhs=ident_f[0:1, 0:1], start=True, stop=True),
            'act': lambda e: e.activation(out=dmy[:, 0:1], in_=dmy[:, 1:2], func=AF.Copy),
            'dve': lambda e: e.memset(dmy[:, 2:3], 0.0),
            'pool': lambda e: e.memset(dmy[:, 3:4], 0.0),
            'sp': lambda e: e.dma_start(out=dmy[0:1, 4:5], in_=din["c_tidx"][0:1, 0:1]),
        }
        SC.barrier(bar_fns)

        ws_cnt = [0]

        def wload(src, view):
            k = ws_cnt[0] % len(WS)
            kf = ws_cnt[0] % len(WSF)
            ws_cnt[0] += 1
            npart = src.shape[0]
            nfree = int(np.prod(src.shape[1:]))
            A('sp', lambda e, kf=kf, src=src, npart=npart, nfree=nfree: e.dma_start(out=WSF[kf][0:npart, 0:nfree], in_=src), writes=[('WSF', kf)], dma=True)
            A('pool', lambda e, k=k, kf=kf, npart=npart, nfree=nfree: e.tensor_copy(out=WS[k][0:npart, 0:nfree], in_=WSF[kf][0:npart, 0:nfree]), reads=[('WSF', kf)], writes=[('WS', k)])
            return k

        def rms_stats(eng_sq, src_ap, col, res_reads):
            A('dve', lambda e: e.memset(stat[:, col:col + 1], 0.0), writes=[('stat', col)])
            A('act', lambda e: e.activation(out=rtmp[:], in_=src_ap, func=AF.Square, accum_out=stat[:, col:col + 1]), reads=list(res_reads) + [('stat', col)], writes=['rtmp', ('stat', col)])
            A('dve', lambda e: e.tensor_scalar(out=stat[:, col:col + 1], in0=stat[:, col:col + 1], scalar1=1.0 / 1024, scalar2=1e-6, op0=ALU.mult, op1=ALU.add), reads=[('stat', col)], writes=[('stat', col)])
            A('act', lambda e: e.activation(out=stat[:, col:col + 1], in_=stat[:, col:col + 1], func=AF.Sqrt), reads=[('stat', col)], writes=[('stat', col)])
            A('dve', lambda e: e.reciprocal(out=stat[:, col:col + 1], in_=stat[:, col:col + 1]), reads=[('stat', col)], writes=[('stat', col)])

        pT_b = pbank[0][:].bitcast(BF16).rearrange("p (a b) -> p a b", a=8)
        pM_b = pbank[1][:].bitcast(BF16)
        pM2_b = pbank[2][:].bitcast(BF16)

        xt_cnt = [0]
        for b in range(NSEQ):
            gate_rows(2, b, 0)
            A('dve', lambda e: e.memset(ksum[:], 0.0), reads=['kmeanT'], writes=['ksum'])
            for cc in range(2):
                A('dve', lambda e, cc=cc: e.memset(car[cc][:], 0.0), writes=[('car', cc)])
            ch_idx = 0
            for g in range(NG):
                t0 = g * GT
                xslots = []
                for j in range(2):
                    xk = xt_cnt[0] % 3
                    xt_cnt[0] += 1
                    xslots.append(xk)
                    ld('sp', XT[xk][:], din["x"][b, t0 + j * 128:t0 + (j + 1) * 128, :], ('XT', xk))
                    rms_stats('act', XT[xk][:], j, [('XT', xk)])
                    A('act', lambda e, xk=xk, j=j: e.activation(out=xs_b[j][:], in_=XT[xk][:], func=AF.Copy, scale=stat[:, j:j + 1]), reads=[('XT', xk), ('stat', j)], writes=[('xs_b', j)])
                    for fc in range(8):
                        A('pe', lambda e, j=j, fc=fc: e.transpose(out=pT_b[:, fc, :], in_=xs_b[j][:, fc * 128:(fc + 1) * 128], identity=ident_b[:]), reads=[('xs_b', j), 'ident_b'], writes=[('ps', 0)])
                    for fc in range(8):
                        A('act', lambda e, j=j, fc=fc, b=b: e.activation(out=hT[:, fc, j * 128:(j + 1) * 128], in_=pT_b[:, fc, :], func=AF.Identity, scale=ABt[:, 0, fc, b:b + 1], bias=ABt[:, 1, fc, b:b + 1]),
                          reads=[('ps', 0), 'ABt'], writes=['hT'])
                SC.mark('g%d_%d_a_norm' % (b, g))
                pcnt = [0]

                def proj_fm(oc, bank):
                    k = wload(din["w_in"][oc].rearrange("p a b -> p (a b)"), [])
                    wv = WS[k][:, :].rearrange("p (a b) -> p a b", a=8)
                    for kc in range(8):
                        A('pe', lambda e, kc=kc, wv=wv, bank=bank: e.matmul(pbank[bank][:, 0:GT], lhsT=wv[:, kc, :], rhs=hT[:, kc, :], start=(kc == 0), stop=(kc == 7)),
                          reads=[('WS', k), 'hT'], writes=[('ps', bank)])

                for hp in range(4):
                    bank = 1 + (pcnt[0] % 2); pcnt[0] += 1
                    proj_fm(hp, bank)
                    A('act', lambda e, hp=hp, bank=bank: e.activation(out=QA[:, hp, :], in_=pbank[bank][:, 0:GT], func=AF.Copy, scale=0.125), reads=[('ps', bank)], writes=['QA'])
                for hp in range(4):
                    bank = 1 + (pcnt[0] % 2); pcnt[0] += 1
                    proj_fm(4 + hp, bank)
                    A('act', lambda e, hp=hp, bank=bank, g=g, t0=t0: e.activation(out=KT[:, hp, t0:t0 + GT], in_=pbank[bank][:, 0:GT], func=AF.Copy, accum_out=ksum[:, hp, g:g + 1]),
                      reads=[('ps', bank)], writes=[('KT', g), 'ksum'])
                for hp in range(4):
                    k = wload(din["w_in"][8 + hp].rearrange("p a b -> p (a b)"), [])
                    wv = WS[k][:, :].rearrange("p (a b) -> p a b", a=8)
                    for j in range(2):
                        bank = 1 + (pcnt[0] % 2); pcnt[0] += 1
                        for kc in range(8):
                            A('pe', lambda e, kc=kc, wv=wv, bank=bank, j=j: e.matmul(pbank[bank][:, 0:128], lhsT=hT[:, kc, j * 128:(j + 1) * 128], rhs=wv[:, kc, :], start=(kc == 0), stop=(kc == 7)),
                              reads=[('WS', k), 'hT'], writes=[('ps', bank)])
                        A('dve', lambda e, bank=bank, j=j, hp=hp, g=g: e.tensor_copy(out=Vt[:, 2 * g + j, 2 * hp:2 * hp + 2, 0:64], in_=pbank[bank][:, 0:128].rearrange("p (a b) -> p a b", a=2)),
                          reads=[('ps', bank)], writes=[('Vt', g)])
                for hf in range(2):
                    bank = 1 + (pcnt[0] % 2); pcnt[0] += 1
                    proj_fm(12 + hf, bank)
                    A('act', lambda e, hf=hf, bank=bank: e.activation(out=uT[:, hf, :], in_=pbank[bank][:, 0:GT], func=AF.Copy), reads=[('ps', bank)], writes=['uT'])
                    A('pool', lambda e, hf=hf: e.tensor_copy(out=uTb[:, hf, :], in_=uT[:, hf, :]), reads=['uT'], writes=['uTb'])

                SC.mark('g%d_%d_b_proj' % (b, g))
                for j in range(2):
                    pgs = [pbank[3 + par][:, 0:64].rearrange("p (a b) -> p a b", a=4) for par in range(2)]
                    for h in range(8):
                        hb, hp, par = (h % 2) * 64, h // 2, h % 2
                        A('pe', lambda e, hb=hb, hp=hp, j=j, pgv=pgs[par]: e.matmul(pgv[:, hp, :], lhsT=QA[hb:hb + 64, hp, j * 128:(j + 1) * 128], rhs=kmeanT[hb:hb + 64, hp, :], start=True, stop=True),
                          reads=['QA', 'kmeanT'], writes=[('ps', 3 + par)])
                    bshape = [128, 8, 16]
                    for par in range(2):
                        A('dve', lambda e, g=g, par=par, pgv=pgs[par]: e.tensor_tensor(out=Gs[:, par * 4:(par + 1) * 4, :], in0=pgv, in1=pneg[:, g, :].unsqueeze(1).to_broadcast([128, 4, 16]), op=ALU.add), reads=[('ps', 3 + par), 'pneg'], writes=['Gs'])
                    A('dve', lambda e: e.tensor_reduce(out=mx[:, 0, :], in_=Gs[:], axis=AX.X, op=ALU.max), reads=['Gs'], writes=['mx0'])
                    A('dve', lambda e: e.tensor_tensor(out=Eq[:], in0=Gs[:], in1=mx[:, 0, :].unsqueeze(2).to_broadcast(bshape), op=ALU.is_ge), reads=['Gs', 'mx0'], writes=['Eq'])
                    A('dve', lambda e: e.scalar_tensor_tensor(out=G2[:], in0=Eq[:], scalar=-BIG, in1=Gs[:], op0=ALU.mult, op1=ALU.add), reads=['Eq', 'Gs'], writes=['G2'])
                    A('dve', lambda e: e.tensor_reduce(out=mx[:, 1, :], in_=G2[:], axis=AX.X, op=ALU.max), reads=['G2'], writes=['mx1'])
                    A('dve', lambda e: e.tensor_tensor(out=Eq[:], in0=G2[:], in1=mx[:, 1, :].unsqueeze(2).to_broadcast(bshape), op=ALU.is_ge), reads=['G2', 'mx1'], writes=['Eq'])
                    A('dve', lambda e: e.scalar_tensor_tensor(out=G2[:], in0=Eq[:], scalar=-BIG, in1=G2[:], op0=ALU.mult, op1=ALU.add), reads=['Eq', 'G2'], writes=['G2'])
                    A('dve', lambda e: e.tensor_reduce(out=mx[:, 2, :], in_=G2[:], axis=AX.X, op=ALU.max), reads=['G2'], writes=['mx2'])
                    A('dve', lambda e: e.tensor_tensor(out=Eq[:], in0=Gs[:], in1=mx[:, 2, :].unsqueeze(2).to_broadcast(bshape), op=ALU.is_ge), reads=['Gs', 'mx2'], writes=['Eq'])
                    A('dve', lambda e, g=g: e.tensor_tensor(out=Eq[:], in0=Eq[:], in1=pastf[:, g, :].unsqueeze(1).to_broadcast(bshape), op=ALU.mult), reads=['Eq', 'pastf'], writes=['Eq'])
                    A('dve', lambda e, g=g: e.tensor_tensor(out=Eq[:], in0=Eq[:], in1=ownf[:, g, :].unsqueeze(1).to_broadcast(bshape), op=ALU.add), reads=['Eq', 'ownf'], writes=['Eq'])
                    A('dve', lambda e: e.tensor_scalar(out=Msb[:], in0=Eq[:], scalar1=-1.0, scalar2=-NEGM, op0=ALU.add, op1=ALU.mult), reads=['Eq'], writes=['Msb'])
                    pmtv = [pM_b[0:16, 0:512].rearrange("p (a b) -> p a b", a=4), pM2_b[64:80, 0:512].rearrange("p (a b) -> p a b", a=4)]
                    for gi in range(8):
                        par = gi // 4
                        A('pe', lambda e, gi=gi, pv=pmtv[par]: e.transpose(out=pv[:, gi % 4, :], in_=Msb[:, gi, :], identity=ident_b[:]), reads=['Msb', 'ident_b'], writes=[('ps', 1 + par)])
                    A('act', lambda e, j=j, pv=pmtv[0]: e.activation(out=MT[0:16, :, j * 128:(j + 1) * 128], in_=pv, func=AF.Copy), reads=[('ps', 1)], writes=['MT'])
                    A('act', lambda e, j=j, pv=pmtv[1]: e.activation(out=MT[64:80, :, j * 128:(j + 1) * 128], in_=pv, func=AF.Copy), reads=[('ps', 2)], writes=['MT'])
                A('dve', lambda e, g=g: e.tensor_scalar(out=kmeanT[:, :, g:g + 1], in0=ksum[:, :, g:g + 1], scalar1=1.0 / 256, scalar2=None, op0=ALU.mult), reads=['ksum', 'QA'], writes=['kmeanT'])

                SC.mark('g%d_%d_c_gate' % (b, g))
                acnt = [0]
                for h in range(8):
                    hb, hp = (h % 2) * 64, h // 2
                    pob = 5 + (h % 2)
                    po = pbank[pob][0:65, 0:GT]
                    nkt = 2 * g + 2
                    for kt in range(nkt):
                        q0 = 128 if kt == 2 * g + 1 else 0
                        sbank = 3 + (acnt[0] % 2)
                        pk = acnt[0] % 3
                        acnt[0] += 1
                        pss = pbank[sbank][:, q0:GT]
                        A('pe', lambda e, hb=hb, hp=hp, kt=kt, q0=q0, pss=pss: e.matmul(pss, lhsT=KT[hb:hb + 64, hp, kt * 128:(kt + 1) * 128], rhs=QA[hb:hb + 64, hp, q0:GT], start=True, stop=False),
                          reads=[('KT', kt // 2), 'QA'], writes=[('ps', sbank)])
                        A('pe', lambda e, h=h, kt=kt, q0=q0, pss=pss: e.matmul(pss, lhsT=eall[(h % 2) * 64:(h % 2) * 64 + 16, kt // 2, :], rhs=MT[(h % 2) * 64:(h % 2) * 64 + 16, h // 2, q0:GT], start=False, stop=True),
                          reads=['eall', 'MT'], writes=[('ps', sbank)])
                        src = pss
                        srcres = ('ps', sbank)
                        if kt >= 2 * g - 1:
                            sk = acnt[0] % 2
                            if kt == 2 * g - 1:
                                A('dve', lambda e, sk=sk, sbank=sbank, h=h: e.tensor_tensor(out=sbias[sk][:, 0:128], in0=pbank[sbank][:, 0:128], in1=B01[:, h, 128:256], op=ALU.add), reads=[('ps', sbank), 'B01'], writes=[('sbias', sk)])
                                A('dve', lambda e, sk=sk, sbank=sbank: e.tensor_copy(out=sbias[sk][:, 128:256], in_=pbank[sbank][:, 128:256]), reads=[('ps', sbank)], writes=[('sbias', sk)])
                            elif kt == 2 * g:
                                A('dve', lambda e, sk=sk, sbank=sbank, h=h: e.tensor_tensor(out=sbias[sk][:, 0:256], in0=pbank[sbank][:, 0:256], in1=B01[:, h, 0:256], op=ALU.add), reads=[('ps', sbank), 'B01'], writes=[('sbias', sk)])
                            else:
                                A('dve', lambda e, sk=sk, sbank=sbank, h=h: e.tensor_tensor(out=sbias[sk][:, 128:256], in0=pbank[sbank][:, 128:256], in1=B01[:, h, 0:128], op=ALU.add), reads=[('ps', sbank), 'B01'], writes=[('sbias', sk)])
                            src = sbias[sk][:, q0:GT]
                            srcres = ('sbias', sk)
                        A('act', lambda e, pk=pk, q0=q0, src=src: e.activation(out=PT[pk][:, q0:GT], in_=src, func=AF.Exp), reads=[srcres], writes=[('PT', pk)])
                        A('pe', lambda e, pk=pk, q0=q0, kt=kt, h=h, pob=pob, nkt=nkt: e.matmul(pbank[pob][0:65, q0:GT], lhsT=Vt[:, kt, h, :], rhs=PT[pk][:, q0:GT], start=(kt == 0), stop=(kt == nkt - 1)),
                          reads=[('PT', pk), ('Vt', kt // 2), 'Vones'], writes=[('ps', pob)])
                    A('dve', lambda e, pob=pob: e.reciprocal(out=rden[64:65, :], in_=pbank[pob][64:65, 0:GT]), reads=[('ps', pob)], writes=['rden'])
                    A('pe', lambda e: e.matmul(pbank[7][0:64, 0:GT], lhsT=ones_f[64:65, 0:64], rhs=rden[64:65, :], start=True, stop=True), reads=['rden', 'ones_f'], writes=[('ps', 7)])
                    A('act', lambda e: e.activation(out=rbc[:], in_=pbank[7][0:64, 0:GT], func=AF.Copy), reads=[('ps', 7)], writes=['rbc'])
                    A('dve', lambda e, h=h, pob=pob: e.tensor_tensor(out=OT[:, h, :], in0=pbank[pob][0:64, 0:GT], in1=rbc[:], op=ALU.mult), reads=[('ps', pob), 'rbc'], writes=['OT'])

                SC.mark('g%d_%d_d_att' % (b, g))
                for c in range(2):
                    cs = c * 128
                    cin = car[ch_idx % 2]
                    cout = car[(ch_idx + 1) % 2]
                    rin = ('car', ch_idx % 2)
                    rout = ('car', (ch_idx + 1) % 2)
                    ch_idx += 1
                    for hf in range(2):
                        for ri in range(2):
                            A('pe', lambda e, hf=hf, ri=ri, cs=cs: e.matmul(pbank[1 + ri][:, :], lhsT=uTb[:, hf, cs:cs + 128], rhs=Bbig[:, hf, ri, :], start=True, stop=True),
                              reads=['uTb', 'Bbig'], writes=[('ps', 1 + ri)])
                        wr = WinvR[:, hf * 512:(hf + 1) * 512]
                        wi = WinvI[:, hf * 512:(hf + 1) * 512]
                        A('dve', lambda e, hf=hf, wr=wr: e.tensor_tensor(out=Uq[0][:, hf, :], in0=pbank[1][:, :], in1=wr, op=ALU.mult), reads=[('ps', 1), 'WinvR_T'], writes=[('Uq', 0, hf)])
                        A('dve', lambda e, hf=hf, wi=wi: e.scalar_tensor_tensor(out=Uq[1][:, hf, :], in0=pbank[2][:, :], scalar=-1.0, in1=wi, op0=ALU.mult, op1=ALU.mult), reads=[('ps', 2), 'WinvI_T'], writes=[('Uq', 1, hf)])
                        A('dve', lambda e, hf=hf, wr=wr: e.tensor_tensor(out=Uq[2][:, hf, :], in0=pbank[2][:, :], in1=wr, op=ALU.mult), reads=[('ps', 2), 'WinvR_T'], writes=[('Uq', 2, hf)])
                        A('dve', lambda e, hf=hf, wi=wi: e.tensor_tensor(out=Uq[3][:, hf, :], in0=pbank[1][:, :], in1=wi, op=ALU.mult), reads=[('ps', 1), 'WinvI_T'], writes=[('Uq', 3, hf)])
                        for ri in range(2):
                            zb = 3 + ri
                            for gl in range(4):
                                for tt in range(2):
                                    ui = ri * 2 + tt
                                    A('pe', lambda e, zb=zb, gl=gl, ui=ui, hf=hf, tt=tt: e.matmul(pbank[zb][:, gl * 128:(gl + 1) * 128], lhsT=Uq[ui][:, hf, gl * 128:(gl + 1) * 128], rhs=tri_b[:], start=(tt == 0), stop=(tt == 1)),
                                      reads=[('Uq', ui, hf), 'tri_b'], writes=[('ps', zb)])
                        zr = pbank[3][:, :].rearrange("p (a b) -> p a b", a=4)
                        zi = pbank[4][:, :].rearrange("p (a b) -> p a b", a=4)
                        gsl = slice(hf * 4, hf * 4 + 4)
                        for gl in range(4):
                            gp = hf * 4 + gl
                            A('dve', lambda e, gl=gl, gp=gp, zr=zr, cin=cin: e.scalar_tensor_tensor(out=TP[:, 0, gp, :], in0=zr[:, gl, :], scalar=cin[:, 0, gp:gp + 1], in1=WpowR[:, gp, :], op0=ALU.add, op1=ALU.mult), reads=[('ps', 3), rin, 'ptab'], writes=[('TP', hf)])
                            A('dve', lambda e, gl=gl, gp=gp, zi=zi, cin=cin: e.scalar_tensor_tensor(out=TP[:, 1, gp, :], in0=zi[:, gl, :], scalar=cin[:, 1, gp:gp + 1], in1=WpowI[:, gp, :], op0=ALU.add, op1=ALU.mult), reads=[('ps', 4), rin, 'ptab'], writes=[('TP', hf)])
                            A('dve', lambda e, gl=gl, gp=gp, zi=zi, cin=cin: e.scalar_tensor_tensor(out=TP[:, 2, gp, :], in0=zi[:, gl, :], scalar=cin[:, 1, gp:gp + 1], in1=WpowR[:, gp, :], op0=ALU.add, op1=ALU.mult), reads=[('ps', 4), rin, 'ptab'], writes=[('TP', hf)])
                            A('dve', lambda e, gl=gl, gp=gp, zr=zr, cin=cin: e.scalar_tensor_tensor(out=TP[:, 3, gp, :], in0=zr[:, gl, :], scalar=cin[:, 0, gp:gp + 1], in1=WpowI[:, gp, :], op0=ALU.add, op1=ALU.mult), reads=[('ps', 3), rin, 'ptab'], writes=[('TP', hf)])
                        A('dve', lambda e, zr=zr, cin=cin, gsl=gsl: e.tensor_tensor(out=ctmp[:, 0, :], in0=zr[:, :, 127], in1=cin[:, 0, gsl], op=ALU.add), reads=[('ps', 3), rin], writes=['ct0'])
                        A('dve', lambda e, zi=zi, cin=cin, gsl=gsl: e.tensor_tensor(out=ctmp[:, 1, :], in0=zi[:, :, 127], in1=cin[:, 1, gsl], op=ALU.add), reads=[('ps', 4), rin], writes=['ct1'])
                        A('dve', lambda e, gsl=gsl: e.tensor_tensor(out=ctmp[:, 2, :], in0=ctmp[:, 0, :], in1=W128[:, 0, gsl], op=ALU.mult), reads=['ct0', 'W128'], writes=['ct2'])
                        A('dve', lambda e, gsl=gsl: e.tensor_tensor(out=ctmp[:, 3, :], in0=ctmp[:, 1, :], in1=W128[:, 1, gsl], op=ALU.mult), reads=['ct1', 'W128'], writes=['ct3'])
                        A('dve', lambda e, gsl=gsl, cout=cout: e.tensor_tensor(out=cout[:, 0, gsl], in0=ctmp[:, 2, :], in1=ctmp[:, 3, :], op=ALU.subtract), reads=['ct2', 'ct3'], writes=[rout])
                        A('dve', lambda e, gsl=gsl: e.tensor_tensor(out=ctmp[:, 4, :], in0=ctmp[:, 1, :], in1=W128[:, 0, gsl], op=ALU.mult), reads=['ct1', 'W128'], writes=['ct4'])
                        A('dve', lambda e, gsl=gsl: e.tensor_tensor(out=ctmp[:, 5, :], in0=ctmp[:, 0, :], in1=W128[:, 1, gsl], op=ALU.mult), reads=['ct0', 'W128'], writes=['ct5'])
                        A('dve', lambda e, gsl=gsl, cout=cout: e.tensor_tensor(out=cout[:, 1, gsl], in0=ctmp[:, 4, :], in1=ctmp[:, 5, :], op=ALU.add), reads=['ct4', 'ct5'], writes=[rout])
                        terms = [(0, 0), (1, 1), (2, 2), (3, 2)]
                        n_mm = 0
                        for gl in range(4):
                            gp = hf * 4 + gl
                            for (ti, ci_) in terms:
                                A('pe', lambda e, gp=gp, ti=ti, ci_=ci_, n_mm=n_mm: e.matmul(pbank[7][:, 0:128], lhsT=Cterm[:, ci_, gp, :], rhs=TP[:, ti, gp, :], start=(n_mm == 0), stop=(n_mm == 15)),
                                  reads=['Cterm', ('TP', hf)], writes=[('ps', 7)])
                                n_mm += 1
                        A('dve', lambda e, hf=hf, cs=cs: e.scalar_tensor_tensor(out=ysb[:], in0=uT[:, hf, cs:cs + 128], scalar=dT[:, hf:hf + 1], in1=pbank[7][:, 0:128], op0=ALU.mult, op1=ALU.add), reads=['uT', 'dT', ('ps', 7)], writes=['ysb'])
                        A('dve', lambda e: e.tensor_tensor(out=ysq[:], in0=ysb[:], in1=ysb[:], op=ALU.mult), reads=['ysb'], writes=['ysq'])
                        A('dve', lambda e: e.tensor_scalar(out=ysq[:], in0=ysq[:], scalar1=0.044715, scalar2=1.0, op0=ALU.mult, op1=ALU.add), reads=['ysq'], writes=['ysq'])
                        A('dve', lambda e: e.tensor_tensor(out=ysq[:], in0=ysq[:], in1=ysb[:], op=ALU.mult), reads=['ysq', 'ysb'], writes=['ysq'])
                        A('act', lambda e: e.activation(out=ysq[:], in_=ysq[:], func=AF.Sigmoid, scale=1.5957691216057308), reads=['ysq'], writes=['ysq'])
                        A('dve', lambda e, hf=hf, cs=cs: e.tensor_tensor(out=zT[:, hf, cs:cs + 128], in0=ysq[:], in1=ysb[:], op=ALU.mult), reads=['ysq', 'ysb'], writes=['zT'])

                SC.mark('g%d_%d_e_ssm' % (b, g))
                for fc in range(8):
                    kA = wload(din["w_ao"][fc].rearrange("p a b -> p (a b)"), [])
                    wa_v = WS[kA][0:64, :].rearrange("p (a b) -> p a b", a=8)
                    for h in range(8):
                        A('pe', lambda e, h=h, wa_v=wa_v: e.matmul(pbank[1][:, 0:GT], lhsT=wa_v[:, h, :], rhs=OT[:, h, :], start=(h == 0), stop=(h == 7)), reads=[('WS', kA), 'OT'], writes=[('ps', 1)])
                    kG = wload(din["w_glu"][fc].rearrange("p a b c -> p (a b c)"), [])
                    wg_v = WS[kG][:, 0:512].rearrange("p (v a b) -> p v a b", v=2, a=2)
                    for vi in range(2):
                        for kc in range(2):
                            A('pe', lambda e, vi=vi, kc=kc, wg_v=wg_v: e.matmul(pbank[2 + 3 * vi][:, 0:GT], lhsT=wg_v[:, vi, kc, :], rhs=zT[:, kc, :], start=(kc == 0), stop=(kc == 1)), reads=[('WS', kG), 'zT'], writes=[('ps', 2 + 3 * vi)])
                    for gi in range(2):
                        oc = 14 + gi * 8 + fc
                        k = wload(din["w_in"][oc].rearrange("p a b -> p (a b)"), [])
                        wv = WS[k][:, :].rearrange("p (a b) -> p a b", a=8)
                        for kc in range(8):
                            A('pe', lambda e, kc=kc, wv=wv, gi=gi: e.matmul(pbank[3 + gi][:, 0:GT], lhsT=wv[:, kc, :], rhs=hT[:, kc, :], start=(kc == 0), stop=(kc == 7)), reads=[('WS', k), 'hT'], writes=[('ps', 3 + gi)])
                    A('act', lambda e: e.activation(out=sgA[0][:], in_=pbank[3][:, 0:GT], func=AF.Sigmoid), reads=[('ps', 3)], writes=[('sgA', 0)])
                    A('act', lambda e: e.activation(out=sgA[1][:], in_=pbank[4][:, 0:GT], func=AF.Sigmoid), reads=[('ps', 4)], writes=[('sgA', 1)])
                    A('act', lambda e: e.activation(out=sgA[2][:], in_=pbank[5][:, 0:GT], func=AF.Sigmoid), reads=[('ps', 5)], writes=[('sgA', 2)])
                    A('dve', lambda e: e.tensor_tensor(out=mt1[:], in0=pbank[1][:, 0:GT], in1=sgA[0][:], op=ALU.mult), reads=[('ps', 1), ('sgA', 0)], writes=['mt1'])
                    A('dve', lambda e: e.tensor_tensor(out=mt2[:], in0=pbank[2][:, 0:GT], in1=sgA[2][:], op=ALU.mult), reads=[('ps', 2), ('sgA', 2)], writes=['mt2'])
                    A('pool', lambda e: e.tensor_tensor(out=mt2[:], in0=mt2[:], in1=sgA[1][:], op=ALU.mult), reads=['mt2', ('sgA', 1)], writes=['mt2'])
                    A('pool', lambda e, fc=fc: e.tensor_tensor(out=mergedT[:, fc, :], in0=mt1[:], in1=mt2[:], op=ALU.add), reads=['mt1', 'mt2'], writes=['mergedT'])

                SC.mark('g%d_%d_f_merge' % (b, g))
                for kc in range(8):
                    k = wload(din["w_mix"][kc], [])
                    for j in range(2):
                        for hh in range(2):
                            bk = 1 + j * 2 + hh
                            A('pe', lambda e, kc=kc, k=k, j=j, hh=hh, bk=bk: e.matmul(pbank[bk][:, :], lhsT=mergedT[:, kc, j * 128:(j + 1) * 128], rhs=WS[k][:, hh * 512:(hh + 1) * 512], start=(kc == 0), stop=(kc == 7)),
                              reads=[('WS', k), 'mergedT'], writes=[('ps', bk)])
                for j in range(2):
                    xk = xslots[j]
                    col = 2 + j
                    A('dve', lambda e, col=col: e.memset(stat[:, col:col + 1], 0.0), writes=[('stat', col)])
                    A('dve', lambda e, col=col: e.memset(stat[:, col + 2:col + 3], 0.0), writes=[('stat', col + 2)])
                    A('act', lambda e, j=j, col=col: e.activation(out=rtmp[:, 0:512], in_=pbank[1 + j * 2][:, :], func=AF.Square, accum_out=stat[:, col:col + 1]), reads=[('ps', 1 + j * 2), ('stat', col)], writes=['rtmp', ('stat', col)])
                    A('act', lambda e, j=j, col=col: e.activation(out=rtmp[:, 512:1024], in_=pbank[2 + j * 2][:, :], func=AF.Square, accum_out=stat[:, col + 2:col + 3]), reads=[('ps', 2 + j * 2), ('stat', col + 2)], writes=['rtmp', ('stat', col + 2)])
                    A('dve', lambda e, col=col: e.tensor_tensor(out=stat[:, col:col + 1], in0=stat[:, col:col + 1], in1=stat[:, col + 2:col + 3], op=ALU.add), reads=[('stat', col), ('stat', col + 2)], writes=[('stat', col)])
                    A('dve', lambda e, col=col: e.tensor_scalar(out=stat[:, col:col + 1], in0=stat[:, col:col + 1], scalar1=1.0 / 1024, scalar2=1e-6, op0=ALU.mult, op1=ALU.add), reads=[('stat', col)], writes=[('stat', col)])
                    A('act', lambda e, col=col: e.activation(out=stat[:, col:col + 1], in_=stat[:, col:col + 1], func=AF.Sqrt), reads=[('stat', col)], writes=[('stat', col)])
                    A('dve', lambda e, col=col: e.reciprocal(out=stat[:, col:col + 1], in_=stat[:, col:col + 1]), reads=[('stat', col)], writes=[('stat', col)])
                    for hh in range(2):
                        bk = 1 + j * 2 + hh
                        A('dve', lambda e, hh=hh, bk=bk, col=col: e.scalar_tensor_tensor(out=rtmp[:, hh * 512:(hh + 1) * 512], in0=pbank[bk][:, :], scalar=stat[:, col:col + 1], in1=Grow[:, hh * 512:(hh + 1) * 512], op0=ALU.mult, op1=ALU.mult),
                          reads=[('ps', bk), ('stat', col), 'Grow'], writes=['rtmp'])
                    A('pool', lambda e, xk=xk: e.tensor_tensor(out=XT[xk][:], in0=XT[xk][:], in1=rtmp[:], op=ALU.add), reads=['rtmp', ('XT', xk)], writes=[('XT', xk)])
                    A('pool', lambda e, xk=xk, b=b, j=j, t0=t0: e.dma_start(out=out[b, t0 + j * 128:t0 + (j + 1) * 128, :], in_=XT[xk][:]), reads=[('XT', xk)], writes=[('out', b, 2 * g + j)], dma=True)


        if do_moe:
            SC.barrier(bar_fns)
            big = (S >= 4096)
            TS = min(1024, S)
            NTS = TS // 128
            if big:
                accv = KT[:].rearrange("p a b -> p (a b)").bitcast(F32)[:, 0:NTS * 1024].rearrange("p (a b) -> p a b", a=NTS)
                vflat = Vt[:].rearrange("p a b c -> p (a b c)")
                h2T = vflat[:, 0:8 * TS].rearrange("p (a b) -> p a b", a=8)
                Wg = vflat[:, 8192:12288].rearrange("p (a b) -> p a b", a=8)
                Wu = vflat[:, 12288:16384].rearrange("p (a b) -> p a b", a=8)
            else:
                accv = sb("m_acc", [128, NTS, 1024], F32)[:]
                h2T = sb("m_h2T", [128, 8, TS], BF16)[:]
                Wg = sb("m_Wg", [128, 8, 512], BF16)[:]
                Wu = sb("m_Wu", [128, 8, 512], BF16)[:]
            hidT = mergedT[:].rearrange("p a b -> p (a b)")[:, 0:2048].rearrange("p (a b) -> p a b", a=4)
            Wg_sets = [Wg, B01[:].rearrange("p a b -> p (a b)").bitcast(BF16).rearrange("p (a b) -> p a b", a=8)]
            Wu_sets = [Wu, TP[:].rearrange("p a b c -> p (a b c)").rearrange("p (a b) -> p a b", a=8)]
            Wd_sets = [[WS[i][:, :] for i in range(4)], [Uq[i][:].rearrange("p a b -> p (a b)") for i in range(4)]]
            STG = [(WSF[i], ('WSF', i)) for i in range(3)] + [(XT[i], ('XT', i)) for i in range(3)]
            stg_cnt = [0]
            Wrt = uTb[:].rearrange("p a b -> p (a b)")[:, 0:288].rearrange("p (a b) -> p a b", a=8)
            Wrt_f = uT[:].rearrange("p a b -> p (a b)")[:, 0:288].rearrange("p (a b) -> p a b", a=8)
            brt = sgA[0][:, 0:36]
            Lg = sgA[0][:, 64:100]
            lem = sgA[1][:, 0:32].rearrange("p (a b) -> p a b", a=4)
            lem2 = sgA[1][:, 32:64]
            e1 = sgA[1][:, 64:96]
            e2 = sgA[1][:, 96:128]
            rs = sgA[2][:, 0:16]
            egt = sgA[2][:, 16:20]
            junk4 = sgA[2][:, 20:24]
            Wtok = sbias[0][:, 0:NTS * 32].rearrange("p (a b) -> p a b", a=NTS)
            ld('sp', Wrt_f[:], din["w_rt"], 'Wrt_f')
            ld('sp', brt[:], bc_rows(din["b_rt"], 128), 'brt')
            A('dve', lambda e: e.tensor_copy(out=Wrt[:], in_=Wrt_f[:]), reads=['Wrt_f'], writes=['Wrt'])
            for b in range(NSEQ):
                gate_rows(5, b, 1)
                for sg in range(S // TS):
                    ts0 = sg * TS
                    A('pool', lambda e: e.memset(accv, 0.0), writes=['acc'])
                    for i in range(NTS):
                        tile = (ts0 // 128) + i
                        xk = xt_cnt[0] % 3
                        xt_cnt[0] += 1
                        ld('sp', XT[xk][:], out[b, tile * 128:(tile + 1) * 128, :], ('XT', xk), reads=[('out', b, tile)])
                        rms_stats('act', XT[xk][:], 0, [('XT', xk)])
                        A('act', lambda e, xk=xk: e.activation(out=xs_b[0][:], in_=XT[xk][:], func=AF.Copy, scale=stat[:, 0:1]), reads=[('XT', xk), ('stat', 0)], writes=[('xs_b', 0)])
                        for fc in range(8):
                            A('pe', lambda e, fc=fc: e.transpose(out=pT_b[:, fc, :], in_=xs_b[0][:, fc * 128:(fc + 1) * 128], identity=ident_b[:]), reads=[('xs_b', 0), 'ident_b'], writes=[('ps', 0)])
                        for fc in range(8):
                            A('act', lambda e, i=i, fc=fc, b=b: e.activation(out=h2T[:, fc, i * 128:(i + 1) * 128], in_=pT_b[:, fc, :], func=AF.Identity, scale=ABt[:, 2, fc, b:b + 1], bias=ABt[:, 3, fc, b:b + 1]),
                              reads=[('ps', 0), 'ABt'], writes=['h2T'])
                        for kc in range(8):
                            A('pe', lambda e, kc=kc, i=i: e.matmul(pbank[3][:, 0:36], lhsT=h2T[:, kc, i * 128:(i + 1) * 128], rhs=Wrt[:, kc, :], start=(kc == 0), stop=(kc == 7)), reads=['h2T', 'Wrt'], writes=[('ps', 3)])
                        A('dve', lambda e: e.tensor_tensor(out=Lg[:], in0=pbank[3][:, 0:36], in1=brt[:], op=ALU.add), reads=[('ps', 3), 'brt'], writes=['Lg'])
                        A('dve', lambda e: e.tensor_reduce(out=rs[:, 0:1], in_=Lg[:, 0:4], axis=AX.X, op=ALU.max), reads=['Lg'], writes=['rs0'])
                        A('dve', lambda e: e.tensor_tensor(out=egt[:], in0=Lg[:, 0:4], in1=rs[:, 0:1].to_broadcast([128, 4]), op=ALU.is_ge), reads=['Lg', 'rs0'], writes=['egt'])
                        A('dve', lambda e: e.tensor_scalar(out=rs[:, 1:2], in0=rs[:, 0:1], scalar1=-1.0, scalar2=None, op0=ALU.mult), reads=['rs0'], writes=['rs1'])
                        A('dve', lambda e: e.memset(rs[:, 2:3], 0.0), writes=['rs2'])
                        A('act', lambda e: e.activation(out=junk4[:], in_=Lg[:, 0:4], func=AF.Exp, bias=rs[:, 1:2], accum_out=rs[:, 2:3]), reads=['Lg', 'rs1', 'rs2'], writes=['rs2', 'junk4'])
                        A('dve', lambda e: e.reciprocal(out=rs[:, 3:4], in_=rs[:, 2:3]), reads=['rs2'], writes=['rs3'])
                        A('dve', lambda e: e.tensor_scalar(out=egt[:], in0=egt[:], scalar1=BIG, scalar2=-BIG, op0=ALU.mult, op1=ALU.add), reads=['egt'], writes=['egt'])
                        A('dve', lambda e: e.tensor_tensor(out=lem[:], in0=Lg[:, 4:36].rearrange("p (a b) -> p a b", a=4), in1=egt[:].unsqueeze(2).to_broadcast([128, 4, 8]), op=ALU.add), reads=['Lg', 'egt'], writes=['lem'])
                        lemf = lem[:].rearrange("p a b -> p (a b)")
                        A('dve', lambda e, lemf=lemf: e.tensor_reduce(out=rs[:, 4:5], in_=lemf, axis=AX.X, op=ALU.max), reads=['lem'], writes=['rs4'])
                        A('dve', lambda e, lemf=lemf: e.tensor_tensor(out=e1[:], in0=lemf, in1=rs[:, 4:5].to_broadcast([128, 32]), op=ALU.is_ge), reads=['lem', 'rs4'], writes=['e1'])
                        A('dve', lambda e, lemf=lemf: e.scalar_tensor_tensor(out=lem2[:], in0=e1[:], scalar=-BIG, in1=lemf, op0=ALU.mult, op1=ALU.add), reads=['e1', 'lem'], writes=['lem2'])
                        A('dve', lambda e: e.tensor_reduce(out=rs[:, 5:6], in_=lem2[:], axis=AX.X, op=ALU.max), reads=['lem2'], writes=['rs5'])
                        A('dve', lambda e: e.tensor_tensor(out=e2[:], in0=lem2[:], in1=rs[:, 5:6].to_broadcast([128, 32]), op=ALU.is_ge), reads=['lem2', 'rs5'], writes=['e2'])
                        A('dve', lambda e: e.tensor_tensor(out=rs[:, 6:7], in0=rs[:, 5:6], in1=rs[:, 4:5], op=ALU.subtract), reads=['rs5', 'rs4'], writes=['rs6'])
                        A('act', lambda e: e.activation(out=rs[:, 7:8], in_=rs[:, 6:7], func=AF.Exp), reads=['rs6'], writes=['rs7'])
                        A('dve', lambda e: e.tensor_scalar(out=rs[:, 8:9], in0=rs[:, 7:8], scalar1=1.0, scalar2=None, op0=ALU.add), reads=['rs7'], writes=['rs8'])
                        A('dve', lambda e: e.reciprocal(out=rs[:, 9:10], in_=rs[:, 8:9]), reads=['rs8'], writes=['rs9'])
                        A('dve', lambda e: e.tensor_tensor(out=rs[:, 10:11], in0=rs[:, 9:10], in1=rs[:, 3:4], op=ALU.mult), reads=['rs9', 'rs3'], writes=['rs10'])
                        A('dve', lambda e: e.tensor_tensor(out=rs[:, 11:12], in0=rs[:, 3:4], in1=rs[:, 10:11], op=ALU.subtract), reads=['rs3', 'rs10'], writes=['rs11'])
                        A('dve', lambda e: e.tensor_scalar(out=e1[:], in0=e1[:], scalar1=rs[:, 10:11], scalar2=None, op0=ALU.mult), reads=['e1', 'rs10'], writes=['e1'])
                        A('dve', lambda e, i=i: e.scalar_tensor_tensor(out=Wtok[:, i, :], in0=e2[:], scalar=rs[:, 11:12], in1=e1[:], op0=ALU.mult, op1=ALU.add), reads=['e2', 'rs11', 'e1'], writes=['Wtok'])
                    for ex in range(32):
                        wsx = ex % 2
                        Wg_c, Wu_c, Wd_c = Wg_sets[wsx], Wu_sets[wsx], Wd_sets[wsx]

                        def wchunk(src, dst, tag, ceng):
                            kf = stg_cnt[0] % len(STG)
                            stg_cnt[0] += 1
                            st_t, st_tag = STG[kf]
                            A('sp', lambda e, st_t=st_t, src=src: e.dma_start(out=st_t[:, :], in_=src), writes=[st_tag], dma=True)
                            if ceng == 'act':
                                A('act', lambda e, st_t=st_t, dst=dst: e.activation(out=dst, in_=st_t[:, :], func=AF.Copy), reads=[st_tag], writes=[tag])
                            else:
                                A('pool', lambda e, st_t=st_t, dst=dst: e.tensor_copy(out=dst, in_=st_t[:, :]), reads=[st_tag], writes=[tag])
                        for c4 in range(4):
                            wchunk(din["w_eg"][ex, :, 2 * c4:2 * c4 + 2, :].rearrange("p a b -> p (a b)"), Wg_c[:, 2 * c4:2 * c4 + 2, :].rearrange("p a b -> p (a b)"), ('Wg', wsx), 'act')
                            wchunk(din["w_eu"][ex, :, 2 * c4:2 * c4 + 2, :].rearrange("p a b -> p (a b)"), Wu_c[:, 2 * c4:2 * c4 + 2, :].rearrange("p a b -> p (a b)"), ('Wu', wsx), 'pool')
                        for c4 in range(4):
                            wchunk(din["w_ed"][ex, :, c4, :], Wd_c[c4], ('Wd', wsx, c4), 'act' if c4 % 2 else 'pool')
                        for q in range(TS // 512):
                            qs = slice(q * 512, (q + 1) * 512)
                            for mc in range(4):
                                bg, bu = 1 + (mc % 2), 3 + (mc % 2)
                                for kc in range(8):
                                    A('pe', lambda e, kc=kc, mc=mc, bg=bg, qs=qs, Wg_c=Wg_c: e.matmul(pbank[bg][:, :], lhsT=Wg_c[:, kc, mc * 128:(mc + 1) * 128], rhs=h2T[:, kc, qs], start=(kc == 0), stop=(kc == 7)), reads=[('Wg', wsx), 'h2T'], writes=[('ps', bg)])
                                for kc in range(8):
                                    A('pe', lambda e, kc=kc, mc=mc, bu=bu, qs=qs, Wu_c=Wu_c: e.matmul(pbank[bu][:, :], lhsT=Wu_c[:, kc, mc * 128:(mc + 1) * 128], rhs=h2T[:, kc, qs], start=(kc == 0), stop=(kc == 7)), reads=[('Wu', wsx), 'h2T'], writes=[('ps', bu)])
                                rt = rtmp[:, (mc % 2) * 512:(mc % 2) * 512 + 512]
                                A('act', lambda e, bg=bg, rt=rt: e.activation(out=rt, in_=pbank[bg][:, :], func=AF.Silu), reads=[('ps', bg)], writes=[('rtmpH', mc % 2)])
                                A('dve', lambda e, bu=bu, rt=rt, mc=mc: e.tensor_tensor(out=hidT[:, mc, :], in0=pbank[bu][:, :], in1=rt, op=ALU.mult), reads=[('ps', bu), ('rtmpH', mc % 2)], writes=['hidT'])
                            for t in range(4):
                                tl = q * 4 + t
                                for hh in range(2):
                                    by = 5 + ((t * 2 + hh) % 3)
                                    for mc in range(4):
                                        A('pe', lambda e, mc=mc, t=t, hh=hh, by=by, Wd_c=Wd_c: e.matmul(pbank[by][:, :], lhsT=hidT[:, mc, t * 128:(t + 1) * 128], rhs=Wd_c[mc][:, hh * 512:(hh + 1) * 512], start=(mc == 0), stop=(mc == 3)), reads=['hidT', ('Wd', wsx, mc)], writes=[('ps', by)])
                                    A('dve', lambda e, tl=tl, hh=hh, by=by, ex=ex: e.scalar_tensor_tensor(out=accv[:, tl, hh * 512:(hh + 1) * 512], in0=pbank[by][:, :], scalar=Wtok[:, tl, ex:ex + 1], in1=accv[:, tl, hh * 512:(hh + 1) * 512], op0=ALU.mult, op1=ALU.add),
                                      reads=[('ps', by), 'Wtok', 'acc'], writes=['acc'])
                    for i in range(NTS):
                        tile = (ts0 // 128) + i
                        xk = xt_cnt[0] % 3
                        xt_cnt[0] += 1
                        ld('sp', XT[xk][:], out[b, tile * 128:(tile + 1) * 128, :], ('XT', xk), reads=[('out', b, tile)])
                        rms_stats('act', accv[:, i, :], 1, ['acc'])
                        A('dve', lambda e, i=i: e.scalar_tensor_tensor(out=rtmp[:], in0=accv[:, i, :], scalar=stat[:, 1:2], in1=Grow[:], op0=ALU.mult, op1=ALU.mult), reads=['acc', ('stat', 1), 'Grow', ('rtmpH', 0), ('rtmpH', 1)], writes=['rtmp', ('rtmpH', 0), ('rtmpH', 1)])
                        A('pool', lambda e, xk=xk: e.tensor_tensor(out=XT[xk][:], in0=XT[xk][:], in1=rtmp[:], op=ALU.add), reads=['rtmp', ('XT', xk)], writes=[('XT', xk)])
                        A('pool', lambda e, xk=xk, b=b, tile=tile: e.dma_start(out=out[b, tile * 128:(tile + 1) * 128, :], in_=XT[xk][:]), reads=[('XT', xk)], writes=[('out', b, tile)], dma=True)

        SC.mark('z_end')
        import os
        if os.environ.get('KSTOP'):
            ks = os.environ['KSTOP']
            SC.ops = SC.ops[:(int(ks) if ks.isdigit() else SC.marks[ks])]
        print('marks', SC.marks, flush=True)
        SC.finalize()
        with nc.Block() as block:
            @block.tensor
            def _(e):
                SC.emit('pe', e)

            @block.scalar
            def _(e):
                SC.emit('act', e)

            @block.vector
            def _(e):
                SC.emit('dve', e)

            @block.gpsimd
            def _(e):
                SC.emit('pool', e)

            @block.sync
            def _(e):
                SC.emit('sp', e)
                SC.final_waits(e)
    return nc


def kernel(**inputs):
    NSEQ, S = 2, 4096
    inp = {k: np.asarray(v) for k, v in inputs.items()}
    w = _layout_weights(inp)
    w.update(_constants())
    nc = build(NSEQ, S)
    in_maps = []
    for c in range(NCORES):
        m = dict(w)
        m["x"] = np.ascontiguousarray(inp["x"][c * NSEQ:(c + 1) * NSEQ])
        m["cT"] = np.ascontiguousarray(inp["c"][c * NSEQ:(c + 1) * NSEQ].reshape(NSEQ, 8, 128).transpose(2, 1, 0))
        in_maps.append(m)
    res = run_bass_kernel_spmd(nc, in_maps, core_ids=list(range(NCORES)))
    return np.concatenate([r["out"] for r in res.results], axis=0).astype(np.float32)
```
